# Optimizing a Trainium2 kernel written in Bass

```python
import jax, jax.numpy as jnp
from jax import lax
import numpy as np

D_MODEL = 2048
BATCH = 4
SEQ = 8192
DEPTH = 1

RNN_WIDTH = D_MODEL
RNN_BLOCKS = 16
CONV_WIDTH = 4
RG_C = 8.0
N_HEADS = 16
HEAD_DIM = D_MODEL // N_HEADS
KV_RANK = 512
IDX_HEADS = 16
IDX_DIM = 64
TOPK_MAX = 256
Q_BLOCK = 128
PEER_HEADS = 8
PEER_KEYS = 128
PEER_EXPERTS = PEER_KEYS * PEER_KEYS
PEER_QDIM = 256
PEER_TOPK = 16
PEER_CHUNK = 128
EPS = 1e-6

SPLITS = (RNN_WIDTH, N_HEADS * HEAD_DIM, KV_RANK, IDX_HEADS * IDX_DIM, IDX_DIM, IDX_HEADS, D_MODEL, D_MODEL)
IN_WIDTH = sum(SPLITS)

kernel_name = "hybrid_rglru_dsa_peer"


def rmsnorm(x, g):
    xf = x.astype(jnp.float32)
    y = xf * lax.rsqrt(jnp.mean(xf * xf, axis=-1, keepdims=True) + EPS)
    return (y * g.astype(jnp.float32)).astype(x.dtype)


def layernorm(x, g, b):
    xf = x.astype(jnp.float32)
    mu = jnp.mean(xf, axis=-1, keepdims=True)
    var = jnp.mean(jnp.square(xf - mu), axis=-1, keepdims=True)
    y = (xf - mu) * lax.rsqrt(var + EPS)
    return (y * g.astype(jnp.float32) + b.astype(jnp.float32)).astype(x.dtype)


def causal_conv(x, w, b):
    s = x.shape[1]
    xp = jnp.pad(x, ((0, 0), (CONV_WIDTH - 1, 0), (0, 0)))
    y = b
    for i in range(CONV_WIDTH):
        y = y + xp[:, i:i + s] * w[i]
    return y


def rg_lru(x, wa, ba, wx, bx, lam):
    bsz, s, _ = x.shape
    xb = x.reshape(bsz, s, RNN_BLOCKS, -1)
    r = jax.nn.sigmoid(jnp.einsum('bsnc,ncd->bsnd', xb, wa).reshape(bsz, s, -1) + ba)
    i = jax.nn.sigmoid(jnp.einsum('bsnc,ncd->bsnd', xb, wx).reshape(bsz, s, -1) + bx)
    log_a = -RG_C * r.astype(jnp.float32) * jax.nn.softplus(-lam.astype(jnp.float32))
    a = jnp.exp(log_a)
    u = jnp.sqrt(-jnp.expm1(2.0 * log_a)) * (i * x).astype(jnp.float32)

    def combine(c1, c2):
        a1, b1 = c1
        a2, b2 = c2
        return a1 * a2, a2 * b1 + b2

    _, h = lax.associative_scan(combine, (a, u), axis=1)
    return h.astype(x.dtype)


def dsa_attention(q, c_kv, q_idx, k_idx, w_idx, w_uk, w_uv):
    bsz, s = c_kv.shape[:2]
    topk = min(TOPK_MAX, s // 4)
    key_pos = jnp.arange(s)

    def block(blk):
        t0 = blk * Q_BLOCK
        sl = lambda a: lax.dynamic_slice_in_dim(a, t0, Q_BLOCK, axis=1)
        qb, qib, wib = sl(q), sl(q_idx), sl(w_idx)
        qpos = t0 + jnp.arange(Q_BLOCK)
        rel = jax.nn.relu(jnp.einsum('bqhd,bsd->bqhs', qib, k_idx) * IDX_DIM ** -0.5)
        iscore = jnp.einsum('bqhs,bqh->bqs', rel, wib).astype(jnp.float32)
        causal = key_pos[None, :] <= qpos[:, None]
        iscore = jnp.where(causal[None], iscore, -jnp.inf)
        _, sel = lax.top_k(iscore, topk)
        valid = sel <= qpos[None, :, None]
        c_sel = jax.vmap(lambda c, ix: c[ix])(c_kv, sel)
        q_lat = jnp.einsum('bqhd,hcd->bqhc', qb, w_uk)
        logits = jnp.einsum('bqhc,bqkc->bqhk', q_lat, c_sel).astype(jnp.float32) * HEAD_DIM ** -0.5
        logits = jnp.where(valid[:, :, None, :], logits, -jnp.inf)
        p = jax.nn.softmax(logits, axis=-1).astype(c_sel.dtype)
        o_lat = jnp.einsum('bqhk,bqkc->bqhc', p, c_sel)
        o = jnp.einsum('bqhc,hcd->bqhd', o_lat, w_uv)
        return o.reshape(bsz, Q_BLOCK, -1)

    out = lax.map(block, jnp.arange(s // Q_BLOCK))
    return out.transpose(1, 0, 2, 3).reshape(bsz, s, -1)


def peer(x, wq, keys1, keys2, u, v):
    bsz, s, d = x.shape
    t = bsz * s
    xf = x.reshape(t, d)
    q = (xf @ wq).reshape(t, PEER_HEADS, PEER_QDIM)
    half = PEER_QDIM // 2
    s1 = jnp.einsum('thd,kd->thk', q[..., :half], keys1)
    s2 = jnp.einsum('thd,kd->thk', q[..., half:], keys2)
    v1, i1 = lax.top_k(s1, PEER_TOPK)
    v2, i2 = lax.top_k(s2, PEER_TOPK)
    cand_s = (v1[..., :, None] + v2[..., None, :]).reshape(t, PEER_HEADS, -1)
    cand_i = (i1[..., :, None] * PEER_KEYS + i2[..., None, :]).reshape(t, PEER_HEADS, -1)
    top_s, pos = lax.top_k(cand_s, PEER_TOPK)
    expert = jnp.take_along_axis(cand_i, pos, axis=-1).reshape(t, -1)
    gate = jax.nn.softmax(top_s.astype(jnp.float32), axis=-1).astype(x.dtype).reshape(t, -1)
    n_chunks = t // PEER_CHUNK

    def chunk(args):
        xc, ec, gc = args
        z = jnp.einsum('cd,ced->ce', xc, u[ec])
        act = jax.nn.gelu(z, approximate=False) * gc
        return jnp.einsum('ce,ced->cd', act, v[ec])

    out = lax.map(chunk, (xf.reshape(n_chunks, PEER_CHUNK, d),
                          expert.reshape(n_chunks, PEER_CHUNK, -1),
                          gate.reshape(n_chunks, PEER_CHUNK, -1)))
    return out.reshape(bsz, s, d)


def setup_inputs(seed: int = 0) -> dict:
    key = jax.random.key(seed)
    ks = jax.random.split(key, 24)
    f32 = jnp.float32
    nrm = lambda k, shape, scale: jax.random.normal(k, shape, f32) * scale
    gain = lambda k, shape: 1.0 + 0.05 * jax.random.normal(k, shape, f32)
    bw = RNN_WIDTH // RNN_BLOCKS
    a_c = jax.random.uniform(ks[9], (DEPTH, RNN_WIDTH), f32, 0.9, 0.999)
    a = a_c ** (1.0 / RG_C)
    rg_lambda = jnp.log(a) - jnp.log1p(-a)
    return {
        "x": nrm(ks[0], (BATCH, SEQ, D_MODEL), 1.0),
        "norm_mix_g": gain(ks[1], (DEPTH, D_MODEL)),
        "w_in": nrm(ks[2], (DEPTH, D_MODEL, IN_WIDTH), D_MODEL ** -0.5),
        "conv_w": nrm(ks[3], (DEPTH, CONV_WIDTH, RNN_WIDTH), CONV_WIDTH ** -0.5),
        "conv_b": nrm(ks[4], (DEPTH, RNN_WIDTH), 0.02),
        "rg_wa": nrm(ks[5], (DEPTH, RNN_BLOCKS, bw, bw), bw ** -0.5),
        "rg_ba": nrm(ks[6], (DEPTH, RNN_WIDTH), 0.1),
        "rg_wx": nrm(ks[7], (DEPTH, RNN_BLOCKS, bw, bw), bw ** -0.5),
        "rg_bx": nrm(ks[8], (DEPTH, RNN_WIDTH), 0.1),
        "rg_lambda": rg_lambda,
        "kv_norm_g": gain(ks[10], (DEPTH, KV_RANK)),
        "w_uk": nrm(ks[11], (DEPTH, N_HEADS, KV_RANK, HEAD_DIM), KV_RANK ** -0.5),
        "w_uv": nrm(ks[12], (DEPTH, N_HEADS, KV_RANK, HEAD_DIM), KV_RANK ** -0.5),
        "idx_ln_g": gain(ks[13], (DEPTH, IDX_DIM)),
        "idx_ln_b": nrm(ks[14], (DEPTH, IDX_DIM), 0.02),
        "w_o": nrm(ks[15], (DEPTH, D_MODEL, D_MODEL), D_MODEL ** -0.5),
        "norm_ffn_g": gain(ks[16], (DEPTH, D_MODEL)),
        "peer_wq": nrm(ks[17], (DEPTH, D_MODEL, PEER_HEADS * PEER_QDIM), D_MODEL ** -0.5),
        "peer_keys1": nrm(ks[18], (DEPTH, PEER_KEYS, PEER_QDIM // 2), (PEER_QDIM // 2) ** -0.5),
        "peer_keys2": nrm(ks[19], (DEPTH, PEER_KEYS, PEER_QDIM // 2), (PEER_QDIM // 2) ** -0.5),
        "peer_u": nrm(ks[20], (DEPTH, PEER_EXPERTS, D_MODEL), D_MODEL ** -0.5),
        "peer_v": nrm(ks[21], (DEPTH, PEER_EXPERTS, D_MODEL), PEER_HEADS ** -0.5),
        "norm_final_g": gain(ks[22], (D_MODEL,)),
    }


def reference(x, norm_mix_g, w_in, conv_w, conv_b, rg_wa, rg_ba, rg_wx, rg_bx, rg_lambda,
              kv_norm_g, w_uk, w_uv, idx_ln_g, idx_ln_b, w_o, norm_ffn_g, peer_wq,
              peer_keys1, peer_keys2, peer_u, peer_v, norm_final_g):
    bsz, s, _ = x.shape
    split_points = [int(p) for p in np.cumsum(SPLITS)[:-1]]
    for l in range(DEPTH):
        h = rmsnorm(x, norm_mix_g[l])
        proj = h @ w_in[l]
        xr, q, ckv, qi, ki, wi, gr, ga = jnp.split(proj, split_points, axis=-1)
        y_rnn = rg_lru(causal_conv(xr, conv_w[l], conv_b[l]),
                       rg_wa[l], rg_ba[l], rg_wx[l], rg_bx[l], rg_lambda[l])
        ckv = rmsnorm(ckv, kv_norm_g[l])
        ki = layernorm(ki, idx_ln_g[l], idx_ln_b[l])
        wi = wi * IDX_HEADS ** -0.5
        y_attn = dsa_attention(q.reshape(bsz, s, N_HEADS, HEAD_DIM), ckv,
                               qi.reshape(bsz, s, IDX_HEADS, IDX_DIM), ki, wi, w_uk[l], w_uv[l])
        mixed = jax.nn.sigmoid(gr) * y_rnn + jax.nn.sigmoid(ga) * y_attn
        x = x + mixed @ w_o[l]
        x = x + peer(rmsnorm(x, norm_ffn_g[l]), peer_wq[l], peer_keys1[l], peer_keys2[l],
                     peer_u[l], peer_v[l])
    return rmsnorm(x, norm_final_g)
```

```python
import numpy as np
from contextlib import ExitStack
import concourse.bass as bass
import concourse.mybir as mybir
from concourse.bass_utils import run_bass_kernel_spmd

F32 = mybir.dt.float32
BF16 = mybir.dt.bfloat16
FP8 = mybir.dt.float8e4
AF = mybir.ActivationFunctionType
ALU = mybir.AluOpType
AX = mybir.AxisListType

D = 2048
NDK = 16
IN_W = 9808
C_XR, C_Q, C_CKV, C_QI, C_KI, C_GR, C_GA = 0, 2048, 4096, 4608, 5632, 5712, 7760
EPS = 1e-6
NEG = -1.0e30
BIGM = 30000.0
NBIS = 18
PEER_MARGIN = 4e-6
SG = 8


class Tok:
    __slots__ = ("w", "r")

    def __init__(self):
        self.w = None
        self.r = []


def toks(n):
    return [Tok() for _ in range(n)]


class Sync:
    NDS = 24

    def __init__(self, nc, es):
        self.nc = nc
        self.eng = {"pe": nc.tensor, "dve": nc.vector, "act": nc.scalar, "pool": nc.gpsimd, "sp": nc.sync}
        self.sem = {}
        self.cnt = {}
        for k in ["pe", "dve", "act", "pool"]:
            self.sem[k] = es.enter_context(nc.semaphore("sem_" + k))
            self.cnt[k] = 0
        for i in range(self.NDS):
            self.sem[("d", i)] = es.enter_context(nc.semaphore("dsem%d" % i))
            self.cnt[("d", i)] = 0
        self.dnext = 0
        self.waited = {k: {} for k in self.eng}
        self.ninstr = 0
        self.enabled = True

    def _wait(self, e, key, val):
        if self.waited[e].get(key, 0) >= val:
            return
        self.eng[e].wait_ge(self.sem[key], val)
        self.waited[e][key] = val

    def _deps(self, e, reads, writes):
        deps = {}
        for t in reads:
            if t.w is not None:
                k, v = t.w
                if deps.get(k, 0) < v:
                    deps[k] = v
        for t in writes:
            if t.w is not None:
                k, v = t.w
                if deps.get(k, 0) < v:
                    deps[k] = v
            for k, v in t.r:
                if deps.get(k, 0) < v:
                    deps[k] = v
        for k, v in deps.items():
            if e == "pe" and k == "pe":
                continue
            self._wait(e, k, v)

    def _commit(self, ev, reads, writes):
        for t in reads:
            t.r.append(ev)
            if len(t.r) > 48:
                best = {}
                for k, v in t.r:
                    if best.get(k, 0) < v:
                        best[k] = v
                t.r = list(best.items())
        for t in writes:
            t.w = ev
            t.r = []

    def op(self, e, fn, reads=(), writes=()):
        if not self.enabled:
            return None
        self._deps(e, reads, writes)
        ins = fn(self.eng[e])
        self.cnt[e] += 1
        ins.then_inc(self.sem[e], 1)
        ev = (e, self.cnt[e])
        self._commit(ev, reads, writes)
        self.ninstr += 1
        return ev

    def dma(self, out, in_, reads=(), writes=(), q="sp"):
        if not self.enabled:
            return None
        slot = self.dnext % self.NDS
        self.dnext += 1
        key = ("d", slot)
        if self.cnt[key] > 0:
            self._wait(q, key, self.cnt[key])
        self._deps(q, reads, writes)
        ins = self.eng[q].dma_start(out=out, in_=in_)
        self.cnt[key] += 16
        ins.then_inc(self.sem[key], 16)
        ev = (key, self.cnt[key])
        self._commit(ev, reads, writes)
        self.ninstr += 1
        return ev

    def barrier(self):
        for e in self.eng:
            for k, v in self.cnt.items():
                if v > 0:
                    self._wait(e, k, v)

    def finish(self, e="sp"):
        for k, v in self.cnt.items():
            if v > 0:
                self._wait(e, k, v)


PH = "PABCDEFG"


def build_program(S, dbg=False):
    NG = S // 1024
    NBLK = S // 128
    NOWN = NBLK // 2
    TOPK = float(min(256, S // 4))
    nc = bass.Bass("TRN2", target_bir_lowering=False)

    def din(name, shape, dt=F32):
        return nc.dram_tensor(name, list(shape), dt, kind="ExternalInput").ap()

    def dscr(name, shape, dt=BF16):
        return nc.dram_tensor(name, list(shape), dt, kind="Internal").ap()

    xs = din("xs", [S, D])
    tri_d = din("tri", [128, 128])
    kb0_d = din("kb0", [128, 128])
    vrow_d = din("vrow", [128, 128])
    rnnp_d = din("rnnp", [128, 8, 16])
    g_mix_d = din("g_mix", [D])
    g_ffn_d = din("g_ffn", [D])
    g_fin_d = din("g_fin", [D])
    g_kv_d = din("g_kv", [512])
    idx_g_d = din("idx_g", [64])
    idx_b_d = din("idx_b", [64])
    w_in_d = din("w_in", [D, IN_W])
    w_o_d = din("w_o", [D, D])
    wq_d = din("wq", [D, D])
    wuk_d = din("wuk", [16, 128, 4, 128])
    wuv_d = din("wuv", [512, D])
    rgwa_d = din("rgwa", [128, 16, 128])
    rgwx_d = din("rgwx", [128, 16, 128])
    k1T_d = din("k1T", [128, 128])
    k2T_d = din("k2T", [128, 128])
    uT_d = din("uT", [128, 128, 16 * 128])
    pv_d = din("pv", [16384, D])
    pow2_d = din("pow2", [128, NBIS + 1])
    y_d = nc.dram_tensor("y", [NOWN * 128, D], F32, kind="ExternalOutput").ap()

    Win_bf = dscr("Win_bf", [D, IN_W])
    Wo_bf = dscr("Wo_bf", [D, D])
    Wq_bf = dscr("Wq_bf", [D, D])
    Wuk_bf = dscr("Wuk_bf", [16, 128, 512])
    Wuv_bf = dscr("Wuv_bf", [512, D])
    UT_bf = dscr("UT_bf", [128, 128, 2048])
    PV_bf = dscr("PV_bf", [16384, D])
    KT_s = dscr("KT_s", [16, 128, S])
    VV_s = dscr("VV_s", [S, D])

    es = ExitStack()
    with es:
        sy = Sync(nc, es)

        uid = {"n": 0}

        def sb(st, name, shape, dt=F32, side=None):
            uid["n"] += 1
            name = "s%d_%s" % (uid["n"], name)
            if side is None:
                return st.enter_context(nc.sbuf_tensor(name, list(shape), dt))
            return st.enter_context(nc.sbuf_tensor(name, list(shape), dt, side=side))

        def psum(st, name, shape, dt=F32):
            uid["n"] += 1
            name = "p%d_%s" % (uid["n"], name)
            return st.enter_context(nc.psum_tensor(name, list(shape), dt))

        rr = {"n": 0}

        def evac(out, in_, reads, writes, engines=("act", "dve")):
            e = engines[rr["n"] % len(engines)]
            rr["n"] += 1
            if e == "act":
                return sy.op("act", lambda en: en.activation(out=out, in_=in_, func=AF.Copy), reads, writes)
            return sy.op(e, lambda en: en.tensor_copy(out=out, in_=in_), reads, writes)

        identf = sb(es, "identf", [128, 128])
        identb = sb(es, "identb", [128, 128], BF16)
        onesb = sb(es, "onesb", [128, 128], BF16)
        tri = sb(es, "tri_s", [128, 128])
        kb0 = sb(es, "kb0_s", [128, 128])
        vrow = sb(es, "vrow_s", [128, 128])
        rnnp = sb(es, "rnnp_s", [128, 8, 16])
        cA = sb(es, "cA", [128, 16])
        cA2 = sb(es, "cA2", [128, 16])
        rgwa = sb(es, "rgwa_s", [128, 16, 128], BF16)
        rgwx = sb(es, "rgwx_s", [128, 16, 128], BF16)
        k1T = sb(es, "k1T_s", [128, 128], BF16)
        k2T = sb(es, "k2T_s", [128, 128], BF16)
        gkvB = sb(es, "gkvB", [128, 512])
        idxgB = sb(es, "idxgB", [128, 64])
        idxbB = sb(es, "idxbB", [128, 64])
        halo = sb(es, "halo", [128, 16, 3])
        hstate = sb(es, "hstate", [128, 16])
        pow2 = sb(es, "pow2_s", [128, NBIS + 1])
        epsT = sb(es, "epsT", [128, 1])
        oneT = sb(es, "oneT", [128, 1])
        kiT2 = sb(es, "kiT2", [128, S], BF16)
        t_c = Tok()
        t_halo = toks(16)
        t_hst = toks(16)
        t_kiT = toks(NBLK)

        with ExitStack() as st:
            stg = [sb(st, "cv_in%d" % i, [128, 2048]) for i in range(3)]
            cvo = [sb(st, "cv_out%d" % i, [128, 2048], BF16) for i in range(3)]
            t_in = toks(3)
            t_out = toks(3)
            CW = 4096
            NCB = 4
            stg2 = [sb(st, "cv2_in%d" % i, [128, CW]) for i in range(NCB)]
            cvo2 = [sb(st, "cv2_out%d" % i, [128, CW], BF16) for i in range(NCB)]
            t_in2 = toks(NCB)
            t_out2 = toks(NCB)
            sy.op("pool", lambda e: e.memset(identf[:], 1.0), writes=[t_c])
            sy.op("pool", lambda e: e.affine_select(out=identf[:], in_=identf[:], pattern=[[-1, 128]],
                                                    compare_op=ALU.is_equal, fill=0.0, base=0,
                                                    channel_multiplier=1), reads=[t_c], writes=[t_c])
            sy.op("pool", lambda e: e.tensor_copy(out=identb[:], in_=identf[:]), reads=[t_c], writes=[t_c])
            sy.op("pool", lambda e: e.memset(onesb[:], 1.0), writes=[t_c])
            sy.op("pool", lambda e: e.memset(epsT[:], EPS), writes=[t_c])
            sy.op("pool", lambda e: e.memset(oneT[:], 1.0), writes=[t_c])
            sy.op("pool", lambda e: e.memset(halo[:], 0.0), writes=t_halo)
            sy.op("pool", lambda e: e.memset(hstate[:], 0.0), writes=t_hst)
            tl = toks(12)
            sy.dma(tri[:], tri_d[:, :], writes=[tl[0]])
            sy.dma(kb0[:], kb0_d[:, :], writes=[tl[1]])
            sy.dma(vrow[:], vrow_d[:, :], writes=[tl[2]])
            sy.dma(rnnp[:], rnnp_d[:, :, :], writes=[tl[3]])
            sy.dma(gkvB[:], g_kv_d.partition_broadcast(128), writes=[tl[4]])
            sy.dma(idxgB[:], idx_g_d.partition_broadcast(128), writes=[tl[5]])
            sy.dma(idxbB[:], idx_b_d.partition_broadcast(128), writes=[tl[6]])
            sy.dma(pow2[:], pow2_d[:, :], writes=[tl[7]])
            for i, (src, dst) in enumerate([(rgwa_d, rgwa), (rgwx_d, rgwx)]):
                sy.dma(stg[i][:, :].rearrange("p (a b) -> p a b", b=128), src[:, :, :], writes=[t_in[i]])
                sy.op("dve", lambda e, i=i, dst=dst: e.tensor_copy(out=dst[:].rearrange("p a b -> p (a b)"), in_=stg[i][:, :]),
                      reads=[t_in[i]], writes=[t_c])
            sy.dma(stg[2][:, 0:128], k1T_d[:, :], writes=[t_in[2]])
            sy.dma(stg[2][:, 128:256], k2T_d[:, :], writes=[t_in[2]])
            sy.op("dve", lambda e: e.tensor_copy(out=k1T[:], in_=stg[2][:, 0:128]), reads=[t_in[2]], writes=[t_c])
            sy.op("dve", lambda e: e.tensor_copy(out=k2T[:], in_=stg[2][:, 128:256]), reads=[t_in[2]], writes=[t_c])
            sy.op("act", lambda e: e.activation(out=cA[:], in_=rnnp[:, 7, :], func=AF.Exp, scale=-1.0), reads=[tl[3]], writes=[t_c])
            sy.op("act", lambda e: e.activation(out=cA[:], in_=cA[:], func=AF.Ln, bias=oneT[:], scale=1.0), reads=[t_c], writes=[t_c])
            sy.op("dve", lambda e: e.tensor_scalar(out=cA2[:], in0=cA[:], scalar1=-16.0, scalar2=None, op0=ALU.mult), reads=[t_c], writes=[t_c])
            sy.op("dve", lambda e: e.tensor_scalar(out=cA[:], in0=cA[:], scalar1=-8.0, scalar2=None, op0=ALU.mult), reads=[t_c], writes=[t_c])
            sy.barrier()

            pieces = []

            def conv_flat(src, dst, numel):
                L = numel // 128
                sv = src.rearrange("(p l) -> p l", p=128)
                dv = dst.rearrange("(p l) -> p l", p=128)
                for c0 in range(0, L, CW):
                    pieces.append((sv, dv, c0, min(CW, L - c0)))
            conv_flat(w_in_d.rearrange("a b -> (a b)"), Win_bf.rearrange("a b -> (a b)"), D * IN_W)
            conv_flat(w_o_d.rearrange("a b -> (a b)"), Wo_bf.rearrange("a b -> (a b)"), D * D)
            conv_flat(wq_d.rearrange("a b -> (a b)"), Wq_bf.rearrange("a b -> (a b)"), D * D)
            conv_flat(wuk_d.rearrange("a b c d -> (a b c d)"), Wuk_bf.rearrange("a b c -> (a b c)"), 16 * 128 * 512)
            conv_flat(wuv_d.rearrange("a b -> (a b)"), Wuv_bf.rearrange("a b -> (a b)"), 512 * D)
            conv_flat(uT_d.rearrange("a b c -> (a b c)"), UT_bf.rearrange("a b c -> (a b c)"), 16384 * D)
            conv_flat(pv_d.rearrange("a b -> (a b)"), PV_bf.rearrange("a b -> (a b)"), 16384 * D)

            def cv_load(k):
                sv, dv, c0, w = pieces[k]
                b = k % NCB
                sy.dma(stg2[b][:, 0:w], sv[:, c0:c0 + w], writes=[t_in2[b]])
            for k in range(min(2, len(pieces))):
                cv_load(k)
            for k, (sv, dv, c0, w) in enumerate(pieces):
                if k + 2 < len(pieces):
                    cv_load(k + 2)
                b = k % NCB
                if k % 2 == 0:
                    sy.op("act", lambda e, b=b, w=w: e.activation(out=cvo2[b][:, 0:w], in_=stg2[b][:, 0:w], func=AF.Copy), reads=[t_in2[b]], writes=[t_out2[b]])
                else:
                    sy.op("dve", lambda e, b=b, w=w: e.tensor_copy(out=cvo2[b][:, 0:w], in_=stg2[b][:, 0:w]), reads=[t_in2[b]], writes=[t_out2[b]])
                sy.dma(dv[:, c0:c0 + w], cvo2[b][:, 0:w], reads=[t_out2[b]])
            sy.barrier()

        Win_v = Win_bf.rearrange("(dk p) c -> p dk c", p=128)
        Wo_v = Wo_bf.rearrange("(dk p) c -> p dk c", p=128)
        Wq_v = Wq_bf.rearrange("(dk p) c -> p dk c", p=128)
        Wuv_v = Wuv_bf.rearrange("(cc p) n -> p cc n", p=128)
        PV_v = PV_bf.rearrange("(i p) n -> p i n", p=128)

        t_KT = [[Tok() for _ in range(16)] for _ in range(NG)]
        t_VV = [[Tok() for _ in range(8)] for _ in range(NG)]

        def rms_rstd(st_ssq, t_ssq, n_feat):
            sy.op("act", lambda e: e.activation(out=st_ssq, in_=st_ssq, func=AF.Sqrt, bias=epsT[:], scale=1.0 / n_feat),
                  reads=[t_ssq, t_c], writes=[t_ssq])
            sy.op("dve", lambda e: e.reciprocal(out=st_ssq, in_=st_ssq), reads=[t_ssq], writes=[t_ssq])

        for g in range(NG):
            with ExitStack() as mix:
                qT = sb(mix, "qT", [128, 16, 512], BF16)
                qiT = sb(mix, "qiT", [128, 8, 512], BF16)
                sga = sb(mix, "sga", [128, 16, 512], BF16)
                mixb = sb(mix, "mixb", [128, 16, 512], BF16)
                absw = sb(mix, "absw", [128, 4, 16])
                sgnw = sb(mix, "sgnw", [128, 4, 16])
                t_qT = toks(16)
                t_qiT = toks(8)
                t_sga = toks(16)
                t_mixb = toks(16)
                t_w = toks(4)
                with ExitStack() as ab:
                    hT = sb(ab, "hT", [128, 16, 1024], BF16)
                    t_hT = toks(8)
                    hTo = hT[:].rearrange("p k (a two t) -> p k a two t", two=2, t=128)
                    pf = [psum(ab, "pfA%d" % i, [128, 512]) for i in range(6)]
                    pb = [psum(ab, "pbA%d" % i, [128, 1024], BF16) for i in range(2)]
                    t_pf = toks(6)
                    t_pb = toks(2)
                    sy.enabled = "A" in PH
                    with ExitStack() as st:
                        xt = [sb(st, "xt%d" % i, [128, D]) for i in range(2)]
                        hb = [sb(st, "hb%d" % i, [128, D], BF16) for i in range(2)]
                        ssq = [sb(st, "ssq%d" % i, [128, 1]) for i in range(2)]
                        gB = sb(st, "gB", [128, D])
                        t_x = toks(2); t_hb = toks(2); t_ssq = toks(2); t_g = Tok()
                        sy.dma(gB[:], g_mix_d.partition_broadcast(128), writes=[t_g])
                        for tt in range(8):
                            b = tt % 2
                            r0 = g * 1024 + tt * 128
                            sy.dma(xt[b][:], xs[r0:r0 + 128, :], writes=[t_x[b]])
                            sy.op("act", lambda e, b=b: e.activation(out=hb[b][:], in_=xt[b][:], func=AF.Square, accum_out=ssq[b][:]),
                                  reads=[t_x[b]], writes=[t_hb[b], t_ssq[b]])
                            rms_rstd(ssq[b][:], t_ssq[b], D)
                            sy.op("dve", lambda e, b=b: e.scalar_tensor_tensor(out=hb[b][:], in0=xt[b][:], scalar=ssq[b][:, 0:1], in1=gB[:],
                                                                              op0=ALU.mult, op1=ALU.mult),
                                  reads=[t_x[b], t_ssq[b], t_g], writes=[t_hb[b]])
                            for k in range(2):
                                for j in range(8):
                                    dk = k * 8 + j
                                    sy.op("pe", lambda e, b=b, k=k, j=j, dk=dk: e.transpose(pb[k][:, j * 128:(j + 1) * 128], hb[b][:, dk * 128:(dk + 1) * 128], identb[:]),
                                          reads=[t_hb[b], t_c], writes=[t_pb[k]])
                                evac(hT[:, k * 8:(k + 1) * 8, tt * 128:(tt + 1) * 128], pb[k][:].rearrange("p (a t) -> p a t", t=128),
                                     [t_pb[k]], [t_hT[tt]])
                        sy.barrier()

                    wst = ExitStack()
                    ab.enter_context(wst)
                    wring = [sb(wst, "wring%d" % i, [128, 16, 256], BF16) for i in range(4)]
                    t_wr = toks(4)
                    wr = {"n": 0}

                    def wload(view, c0, ncols=256):
                        i = wr["n"] % 4
                        wr["n"] += 1
                        sy.dma(wring[i][:, :, 0:ncols], view[:, :, c0:c0 + ncols], writes=[t_wr[i]])
                        return wring[i], t_wr[i]

                    pfr = {"n": 0}

                    def next_pf(lo=0, hi=6):
                        i = lo + pfr["n"] % (hi - lo)
                        pfr["n"] += 1
                        return pf[i], t_pf[i]

                    sy.enabled = "B" in PH
                    with ExitStack() as st:
                        NB_ = 3
                        xpad = [sb(st, "xpad%d" % i, [128, 515]) for i in range(NB_)]
                        xc = [sb(st, "xc%d" % i, [128, 512]) for i in range(NB_)]
                        xcb = [sb(st, "xcb%d" % i, [128, 512], BF16) for i in range(NB_)]
                        rr_ = [sb(st, "rg_r%d" % i, [128, 512]) for i in range(NB_)]
                        ri_ = [sb(st, "rg_i%d" % i, [128, 512]) for i in range(NB_)]
                        ra_ = [sb(st, "rg_a%d" % i, [128, 512]) for i in range(NB_)]
                        ra2_ = [sb(st, "rg_a2%d" % i, [128, 512]) for i in range(NB_)]
                        hsq = [sb(st, "hsq%d" % i, [128, 512]) for i in range(NB_)]
                        sgr = [sb(st, "sgr%d" % i, [128, 256]) for i in range(NB_)]
                        t_xpad = toks(NB_); t_xc = toks(NB_); t_xcb = toks(NB_); t_r = toks(NB_); t_i = toks(NB_)
                        t_a = toks(NB_); t_a2 = toks(NB_); t_hsq = toks(NB_); t_sgr = toks(NB_)
                        iters = [(cp, cc, hf) for cp in range(8) for cc in range(2) for hf in range(2)]
                        wcache = {}
                        st_ = {}

                        def rnn_front(k):
                            cp, cc, hf = iters[k]
                            c = cp * 2 + cc
                            b = k % NB_
                            if cp not in wcache:
                                wcache[cp] = (wload(Win_v, C_XR + cp * 256), wload(Win_v, C_GR + cp * 256))
                            (wx, t_wx), (wg, t_wg) = wcache[cp]
                            p_x, t_px = next_pf()
                            for dk in range(16):
                                sy.op("pe", lambda e, dk=dk: e.matmul(p_x[:], lhsT=wx[:, dk, cc * 128:(cc + 1) * 128], rhs=hT[:, dk, hf * 512:(hf + 1) * 512],
                                                                      start=(dk == 0), stop=(dk == 15)),
                                      reads=[t_wx] + t_hT[hf * 4:(hf + 1) * 4], writes=[t_px])
                            p_g, t_pg = next_pf()
                            for dk in range(16):
                                sy.op("pe", lambda e, dk=dk: e.matmul(p_g[:, 0:256].rearrange("p (a t) -> p a t", t=128), lhsT=wg[:, dk, cc * 128:(cc + 1) * 128],
                                                                      rhs=hTo[:, dk, hf * 2:(hf + 1) * 2, 1, :], start=(dk == 0), stop=(dk == 15)),
                                      reads=[t_wg] + t_hT[hf * 4:(hf + 1) * 4], writes=[t_pg])
                            sy.op("dve", lambda e: e.tensor_copy(out=xpad[b][:, 0:3], in_=halo[:, c, :]), reads=[t_halo[c]], writes=[t_xpad[b]])
                            sy.op("act", lambda e: e.activation(out=xpad[b][:, 3:515], in_=p_x[:], func=AF.Copy), reads=[t_px], writes=[t_xpad[b]])
                            sy.op("dve", lambda e: e.tensor_copy(out=halo[:, c, :], in_=xpad[b][:, 512:515]), reads=[t_xpad[b]], writes=[t_halo[c]])
                            sy.op("dve", lambda e: e.tensor_scalar(out=xc[b][:], in0=xpad[b][:, 3:515], scalar1=rnnp[:, 3, c:c + 1], scalar2=rnnp[:, 4, c:c + 1],
                                                                   op0=ALU.mult, op1=ALU.add), reads=[t_xpad[b], t_c], writes=[t_xc[b]])
                            for kk in range(3):
                                sy.op("dve", lambda e, kk=kk: e.scalar_tensor_tensor(out=xc[b][:], in0=xpad[b][:, kk:kk + 512], scalar=rnnp[:, kk, c:c + 1], in1=xc[b][:],
                                                                                   op0=ALU.mult, op1=ALU.add), reads=[t_xpad[b], t_xc[b]], writes=[t_xc[b]])
                            sy.op("act", lambda e: e.activation(out=xcb[b][:], in_=xc[b][:], func=AF.Copy), reads=[t_xc[b]], writes=[t_xcb[b]])
                            sy.op("act", lambda e: e.activation(out=sgr[b][:], in_=p_g[:, 0:256], func=AF.Sigmoid), reads=[t_pg], writes=[t_sgr[b]])

                        def rnn_back(k):
                            cp, cc, hf = iters[k]
                            c = cp * 2 + cc
                            b = k % NB_
                            p_r, t_pr = next_pf()
                            sy.op("pe", lambda e: e.matmul(p_r[:], lhsT=rgwa[:, c, :], rhs=xcb[b][:], start=True, stop=True), reads=[t_xcb[b], t_c], writes=[t_pr])
                            p_i, t_pi = next_pf()
                            sy.op("pe", lambda e: e.matmul(p_i[:], lhsT=rgwx[:, c, :], rhs=xcb[b][:], start=True, stop=True), reads=[t_xcb[b], t_c], writes=[t_pi])
                            sy.op("act", lambda e: e.activation(out=rr_[b][:], in_=p_r[:], func=AF.Sigmoid, bias=rnnp[:, 5, c:c + 1], scale=1.0), reads=[t_pr, t_c], writes=[t_r[b]])
                            sy.op("act", lambda e: e.activation(out=ri_[b][:], in_=p_i[:], func=AF.Sigmoid, bias=rnnp[:, 6, c:c + 1], scale=1.0), reads=[t_pi, t_c], writes=[t_i[b]])
                            sy.op("act", lambda e: e.activation(out=ra_[b][:], in_=rr_[b][:], func=AF.Exp, scale=cA[:, c:c + 1]), reads=[t_r[b], t_c], writes=[t_a[b]])
                            sy.op("act", lambda e: e.activation(out=ra2_[b][:], in_=rr_[b][:], func=AF.Exp, scale=cA2[:, c:c + 1]), reads=[t_r[b], t_c], writes=[t_a2[b]])
                            sy.op("act", lambda e: e.activation(out=ra2_[b][:], in_=ra2_[b][:], func=AF.Sqrt, bias=oneT[:], scale=-1.0), reads=[t_a2[b], t_c], writes=[t_a2[b]])
                            sy.op("dve", lambda e: e.tensor_tensor(out=ri_[b][:], in0=ri_[b][:], in1=xc[b][:], op=ALU.mult), reads=[t_i[b], t_xc[b]], writes=[t_i[b]])
                            sy.op("dve", lambda e: e.tensor_tensor(out=ri_[b][:], in0=ri_[b][:], in1=ra2_[b][:], op=ALU.mult), reads=[t_i[b], t_a2[b]], writes=[t_i[b]])
                            if g == 0 and hf == 0:
                                sy.op("dve", lambda e: e.tensor_tensor(out=ri_[b][:, 0:128], in0=ri_[b][:, 0:128], in1=vrow[:], op=ALU.mult), reads=[t_i[b], t_c], writes=[t_i[b]])
                            sy.op("dve", lambda e: e.tensor_tensor_scan(out=hsq[b][:], data0=ra_[b][:], data1=ri_[b][:], initial=hstate[:, c:c + 1], op0=ALU.mult, op1=ALU.add),
                                  reads=[t_a[b], t_i[b], t_hst[c]], writes=[t_hsq[b]])
                            sy.op("dve", lambda e: e.tensor_copy(out=hstate[:, c:c + 1], in_=hsq[b][:, 511:512]), reads=[t_hsq[b]], writes=[t_hst[c]])
                            sy.op("dve", lambda e: e.tensor_tensor(out=mixb[:, c, hf * 256:(hf + 1) * 256].rearrange("p (a t) -> p a t", t=128),
                                                                   in0=sgr[b][:].rearrange("p (a t) -> p a t", t=128),
                                                                   in1=hsq[b][:].rearrange("p (a two t) -> p a two t", two=2, t=128)[:, :, 1, :], op=ALU.mult),
                                  reads=[t_sgr[b], t_hsq[b]], writes=[t_mixb[c]])

                        rnn_front(0)
                        rnn_front(1)
                        for k in range(len(iters)):
                            if k + 2 < len(iters):
                                rnn_front(k + 2)
                            rnn_back(k)
                        sy.barrier()

                    sy.enabled = "b" in PH or "B" in PH
                    with ExitStack() as st:
                        ckvT = sb(st, "ckvT", [128, 4, 1024], BF16)
                        t_ckvT = toks(8)
                        ckvn = [sb(st, "ckvn%d" % i, [128, 512], BF16) for i in range(2)]
                        sq2 = [sb(st, "sq2_%d" % i, [128, 1]) for i in range(2)]
                        junk = sb(st, "junkB", [128, 512], BF16)
                        kcen = [sb(st, "kcen%d" % i, [128, 64]) for i in range(2)]
                        kin2 = [sb(st, "kin2_%d" % i, [128, 128], BF16) for i in range(2)]
                        ksm = [sb(st, "ksm%d" % i, [128, 4]) for i in range(2)]
                        wI = sb(st, "wI", [128, 4, 16])
                        wuk = [sb(st, "wuk%d" % i, [128, 4, 128], BF16) for i in range(2)]
                        wuv = sb(st, "wuv", [128, 4, D], BF16)
                        ktw = [sb(st, "ktw%d" % i, [128, 1024], BF16) for i in range(2)]
                        vw = [sb(st, "vw%d" % i, [128, D], BF16) for i in range(2)]
                        t_ckvn = toks(2); t_sq2 = toks(2); t_junk = Tok(); t_kcen = toks(2); t_kin2 = toks(2); t_ksm = toks(2)
                        t_wuk = toks(2); t_wuv = Tok(); t_ktw = toks(2); t_vw = toks(2)
                        sy.dma(wuv[:], Wuv_v[:, :, :], writes=[t_wuv])

                        def fm_proj(c_base, nchunks, dst, t_dst, sig):
                            for cp in range(nchunks // 2):
                                wb, t_wb = wload(Win_v, c_base + cp * 256)
                                for cc in range(2):
                                    c = cp * 2 + cc
                                    p_, t_p = next_pf()
                                    for dk in range(16):
                                        sy.op("pe", lambda e, dk=dk, p_=p_, wb=wb, cc=cc: e.matmul(p_[:].rearrange("p (a t) -> p a t", t=128), lhsT=wb[:, dk, cc * 128:(cc + 1) * 128],
                                                                                      rhs=hTo[:, dk, :, 1, :], start=(dk == 0), stop=(dk == 15)),
                                              reads=[t_wb] + t_hT, writes=[t_p])
                                    if sig:
                                        sy.op("act", lambda e, p_=p_, c=c: e.activation(out=dst[:, c, :], in_=p_[:], func=AF.Sigmoid), reads=[t_p], writes=[t_dst[c]])
                                    else:
                                        evac(dst[:, c, :], p_[:], [t_p], [t_dst[c]])
                        fm_proj(C_Q, 16, qT, t_qT, False)
                        fm_proj(C_QI, 8, qiT, t_qiT, False)
                        fm_proj(C_GA, 16, sga, t_sga, True)

                        wc0, t_wc0 = wload(Win_v, C_CKV)
                        wc1, t_wc1 = wload(Win_v, C_CKV + 256)
                        for tt in range(8):
                            b = tt % 2
                            p_, t_p = next_pf()
                            for pc, (wc, t_wc) in enumerate([(wc0, t_wc0), (wc1, t_wc1)]):
                                for dk in range(16):
                                    sy.op("pe", lambda e, dk=dk, p_=p_, wc=wc, pc=pc, tt=tt: e.matmul(p_[:, pc * 256:(pc + 1) * 256], lhsT=hT[:, dk, tt * 128:(tt + 1) * 128], rhs=wc[:, dk, :],
                                                                                        start=(dk == 0), stop=(dk == 15)),
                                          reads=[t_wc, t_hT[tt]], writes=[t_p])
                            sy.op("act", lambda e, p_=p_, b=b: e.activation(out=junk[:], in_=p_[:], func=AF.Square, accum_out=sq2[b][:]), reads=[t_p], writes=[t_junk, t_sq2[b]])
                            rms_rstd(sq2[b][:], t_sq2[b], 512)
                            sy.op("dve", lambda e, p_=p_, b=b: e.scalar_tensor_tensor(out=ckvn[b][:], in0=p_[:], scalar=sq2[b][:, 0:1], in1=gkvB[:], op0=ALU.mult, op1=ALU.mult),
                                  reads=[t_p, t_sq2[b], t_c], writes=[t_ckvn[b]])
                            k = tt % 2
                            for cc in range(4):
                                sy.op("pe", lambda e, b=b, k=k, cc=cc: e.transpose(pb[k][:, cc * 128:(cc + 1) * 128], ckvn[b][:, cc * 128:(cc + 1) * 128], identb[:]),
                                      reads=[t_ckvn[b], t_c], writes=[t_pb[k]])
                            evac(ckvT[:, :, tt * 128:(tt + 1) * 128], pb[k][:, 0:512].rearrange("p (a t) -> p a t", t=128), [t_pb[k]], [t_ckvT[tt]])

                        wk, t_wk = wload(Win_v, C_KI, 80)
                        for tt in range(8):
                            b = tt % 2
                            p_, t_p = next_pf()
                            for dk in range(16):
                                sy.op("pe", lambda e, dk=dk, p_=p_, tt=tt: e.matmul(p_[:, 0:80], lhsT=hT[:, dk, tt * 128:(tt + 1) * 128], rhs=wk[:, dk, 0:80], start=(dk == 0), stop=(dk == 15)),
                                      reads=[t_wk, t_hT[tt]], writes=[t_p])
                            sy.op("dve", lambda e, p_=p_, b=b: e.tensor_reduce(out=ksm[b][:, 0:1], in_=p_[:, 0:64], axis=AX.X, op=ALU.add), reads=[t_p], writes=[t_ksm[b]])
                            sy.op("dve", lambda e, b=b: e.tensor_scalar(out=ksm[b][:, 1:2], in0=ksm[b][:, 0:1], scalar1=-1.0 / 64.0, scalar2=None, op0=ALU.mult), reads=[t_ksm[b]], writes=[t_ksm[b]])
                            sy.op("dve", lambda e, p_=p_, b=b: e.tensor_scalar(out=kcen[b][:], in0=p_[:, 0:64], scalar1=ksm[b][:, 1:2], scalar2=None, op0=ALU.add), reads=[t_p, t_ksm[b]], writes=[t_kcen[b]])
                            sy.op("act", lambda e, b=b: e.activation(out=junk[:, 0:64], in_=kcen[b][:], func=AF.Square, accum_out=ksm[b][:, 2:3]), reads=[t_kcen[b]], writes=[t_junk, t_ksm[b]])
                            rms_rstd(ksm[b][:, 2:3], t_ksm[b], 64)
                            sy.op("dve", lambda e, b=b: e.scalar_tensor_tensor(out=kcen[b][:], in0=kcen[b][:], scalar=ksm[b][:, 2:3], in1=idxgB[:], op0=ALU.mult, op1=ALU.mult),
                                  reads=[t_kcen[b], t_ksm[b], t_c], writes=[t_kcen[b]])
                            sy.op("dve", lambda e, b=b: e.tensor_tensor(out=kin2[b][:, 0:64], in0=kcen[b][:], in1=idxbB[:], op=ALU.add), reads=[t_kcen[b], t_c], writes=[t_kin2[b]])
                            sy.op("dve", lambda e, b=b: e.tensor_tensor(out=kin2[b][:, 64:128], in0=kcen[b][:], in1=idxbB[:], op=ALU.add), reads=[t_kcen[b], t_c], writes=[t_kin2[b]])
                            k = tt % 2
                            sy.op("pe", lambda e, b=b, k=k: e.transpose(pb[k][:, 0:128], kin2[b][:], identb[:]), reads=[t_kin2[b], t_c], writes=[t_pb[k]])
                            blk = g * 8 + tt
                            evac(kiT2[:, blk * 128:(blk + 1) * 128], pb[k][:, 0:128], [t_pb[k]], [t_kiT[blk]])
                            if tt % 2 == 1:
                                jj = tt // 2
                                sy.op("dve", lambda e, p_=p_, jj=jj: e.tensor_scalar(out=wI[:, jj, :], in0=p_[:, 64:80], scalar1=0.25 * 0.125, scalar2=None, op0=ALU.mult), reads=[t_p], writes=[t_w[jj]])
                                sy.op("act", lambda e, jj=jj: e.activation(out=absw[:, jj, :], in_=wI[:, jj, :], func=AF.Abs), reads=[t_w[jj]], writes=[t_w[jj]])
                                sy.op("act", lambda e, jj=jj: e.activation(out=sgnw[:, jj, :], in_=wI[:, jj, :], func=AF.Sign), reads=[t_w[jj]], writes=[t_w[jj]])

                        for h in range(16):
                            b = h % 2
                            sy.dma(wuk[b][:].rearrange("p a b -> p (a b)"), Wuk_bf[h, :, :], writes=[t_wuk[b]])
                            for nt in range(2):
                                p_, t_p = next_pf()
                                for cc in range(4):
                                    sy.op("pe", lambda e, p_=p_, b=b, cc=cc, nt=nt: e.matmul(p_[:], lhsT=wuk[b][:, cc, :], rhs=ckvT[:, cc, nt * 512:(nt + 1) * 512], start=(cc == 0), stop=(cc == 3)),
                                          reads=[t_wuk[b]] + t_ckvT[nt * 4:(nt + 1) * 4], writes=[t_p])
                                evac(ktw[b][:, nt * 512:(nt + 1) * 512], p_[:], [t_p], [t_ktw[b]])
                            sy.dma(KT_s[h, :, g * 1024:(g + 1) * 1024], ktw[b][:], reads=[t_ktw[b]], writes=[t_KT[g][h]])
                        for stl in range(8):
                            b = stl % 2
                            for nb in range(4):
                                p_, t_p = next_pf()
                                for cc in range(4):
                                    sy.op("pe", lambda e, p_=p_, cc=cc, nb=nb, stl=stl: e.matmul(p_[:], lhsT=ckvT[:, cc, stl * 128:(stl + 1) * 128], rhs=wuv[:, cc, nb * 512:(nb + 1) * 512], start=(cc == 0), stop=(cc == 3)),
                                          reads=[t_wuv, t_ckvT[stl]], writes=[t_p])
                                evac(vw[b][:, nb * 512:(nb + 1) * 512], p_[:], [t_p], [t_vw[b]])
                            r0 = g * 1024 + stl * 128
                            sy.dma(VV_s[r0:r0 + 128, :], vw[b][:], reads=[t_vw[b]], writes=[t_VV[g][stl]])
                        sy.barrier()
                    sy.barrier()

                sy.enabled = "C" in PH
                NST = 8 * g + 8
                with ExitStack() as cd:
                    maskbT = sb(cd, "maskbT", [128, NST, 512], BF16)
                    t_mb = [[Tok() for _ in range(4)] for _ in range(NST // 8)]
                    pf = [psum(cd, "pfC%d" % i, [128, 512]) for i in range(6)]
                    pb = [psum(cd, "pbC%d" % i, [128, 1024], BF16) for i in range(2)]
                    t_pf = toks(6)
                    t_pb = toks(2)
                    with ExitStack() as st:
                        isc = sb(st, "isc", [128, NST * 128])
                        junk8 = sb(st, "junk8", [128, NST * 128], FP8)
                        maskq = [sb(st, "maskq%d" % i, [128, 1024], BF16) for i in range(2)]
                        rtmp = [sb(st, "rtmp%d" % i, [128, 1024]) for i in range(2)]
                        bis = sb(st, "bis", [128, 8])
                        half = sb(st, "half", [128, NBIS + 1])
                        bis2 = sb(st, "bis2", [128, 2])
                        t_mid = Tok(); t_cnt = Tok(); t_cnt2 = Tok(); t_comb = Tok(); t_junk8b = Tok()
                        t_isc = Tok(); t_junk8 = Tok(); t_mq = toks(2); t_rt = toks(2); t_bis = Tok(); t_half = Tok()
                        rti = 0
                        pfi = 0
                        for jj in range(4):
                            pblk = 8 * g + 2 * jj + 1
                            nk = (pblk + 1) * 128
                            nts = [(nt, min(512, nk - nt * 512)) for nt in range((nk + 511) // 512)]
                            for pr in range(0, len(nts), 2):
                                grp = nts[pr:pr + 2]
                                c0 = grp[0][0] * 512
                                wt = sum(w_ for _, w_ in grp)
                                for h in range(16):
                                    hp = (h % 2) * 64
                                    rb = rti % 2
                                    rti += 1
                                    off = 0
                                    for (nt, w) in grp:
                                        p_, t_p = pf[pfi % 6], t_pf[pfi % 6]
                                        pfi += 1
                                        kblks = t_kiT[nt * 4:nt * 4 + w // 128]
                                        sy.op("pe", lambda e, p_=p_, nt=nt, w=w: e.matmul(p_[:, 0:w], lhsT=qiT[hp:hp + 64, h // 2, jj * 128:(jj + 1) * 128],
                                                                               rhs=kiT2[hp:hp + 64, nt * 512:nt * 512 + w], start=True, stop=True),
                                              reads=[t_qiT[h // 2]] + kblks, writes=[t_p])
                                        sy.op("act", lambda e, p_=p_, off=off, w=w: e.activation(out=rtmp[rb][:, off:off + w], in_=p_[:, 0:w], func=AF.Relu, scale=absw[:, jj, h:h + 1]),
                                              reads=[t_p, t_w[jj]], writes=[t_rt[rb]])
                                        off += w
                                    if h == 0:
                                        sy.op("dve", lambda e: e.tensor_scalar(out=isc[:, c0:c0 + wt], in0=rtmp[rb][:, 0:wt], scalar1=sgnw[:, jj, 0:1], scalar2=None, op0=ALU.mult),
                                              reads=[t_rt[rb], t_w[jj]], writes=[t_isc])
                                    else:
                                        sy.op("dve", lambda e: e.scalar_tensor_tensor(out=isc[:, c0:c0 + wt], in0=rtmp[rb][:, 0:wt], scalar=sgnw[:, jj, h:h + 1],
                                                                                     in1=isc[:, c0:c0 + wt], op0=ALU.mult, op1=ALU.add),
                                              reads=[t_rt[rb], t_w[jj], t_isc], writes=[t_isc])
                            sy.op("dve", lambda e, nk=nk: e.tensor_reduce(out=bis[:, 0:1], in_=isc[:, 0:nk], axis=AX.X, op=ALU.max, apply_absolute_value=True), reads=[t_isc], writes=[t_bis])
                            sy.op("dve", lambda e: e.tensor_scalar(out=bis[:, 1:2], in0=bis[:, 0:1], scalar1=2.0, scalar2=2.0, op0=ALU.mult, op1=ALU.add), reads=[t_bis], writes=[t_bis])
                            sy.op("dve", lambda e: e.tensor_scalar(out=half[:], in0=pow2[:], scalar1=bis[:, 1:2], scalar2=None, op0=ALU.mult), reads=[t_bis, t_c], writes=[t_half])
                            sy.op("dve", lambda e: e.tensor_scalar(out=bis[:, 2:3], in0=bis[:, 1:2], scalar1=-0.5, scalar2=half[:, 0:1], op0=ALU.mult, op1=ALU.add), reads=[t_bis, t_half], writes=[t_mid])
                            sy.op("dve", lambda e, pblk=pblk: e.tensor_tensor(out=isc[:, pblk * 128:(pblk + 1) * 128], in0=isc[:, pblk * 128:(pblk + 1) * 128], in1=tri[:], op=ALU.add), reads=[t_isc, t_c], writes=[t_isc])
                            if g == 0:
                                sy.op("dve", lambda e: e.tensor_tensor(out=isc[:, 0:128], in0=isc[:, 0:128], in1=kb0[:], op=ALU.add), reads=[t_isc, t_c], writes=[t_isc])
                            n1 = ((nk // 2 + 127) // 128) * 128
                            n2 = nk - n1
                            for k in range(NBIS):
                                sy.op("dve", lambda e: e.tensor_scalar(out=junk8[:, 0:n1], in0=isc[:, 0:n1], scalar1=bis[:, 2:3], scalar2=None, op0=ALU.is_ge, op1=ALU.add, accum_out=bis[:, 3:4], saturate=False),
                                      reads=[t_isc, t_mid], writes=[t_junk8, t_cnt])
                                sy.op("act", lambda e: e.activation(out=junk8[:, n1:nk], in_=isc[:, n1:nk], func=AF.Sign, bias=bis[:, 2:3], scale=-1.0, accum_out=bis2[:, 0:1], saturate=False),
                                      reads=[t_isc, t_mid], writes=[t_junk8b, t_cnt2])
                                kk = k + 1 if k < NBIS - 1 else k
                                sy.op("dve", lambda e, kk=kk: e.tensor_tensor(out=bis[:, 4:5], in0=bis[:, 2:3], in1=half[:, kk:kk + 1], op=ALU.subtract), reads=[t_mid, t_half], writes=[t_bis])
                                sy.op("dve", lambda e: e.scalar_tensor_tensor(out=bis[:, 6:7], in0=bis[:, 3:4], scalar=2.0, in1=bis2[:, 0:1], op0=ALU.mult, op1=ALU.subtract),
                                      reads=[t_cnt, t_cnt2], writes=[t_comb])
                                sy.op("dve", lambda e, k=k: e.scalar_tensor_tensor(out=bis[:, 5:6], in0=bis[:, 6:7], scalar=2.0 * TOPK - n2, in1=half[:, k:k + 1], op0=ALU.is_ge, op1=ALU.mult),
                                      reads=[t_comb, t_half], writes=[t_bis])
                                sy.op("dve", lambda e: e.tensor_tensor(out=bis[:, 2:3], in0=bis[:, 4:5], in1=bis[:, 5:6], op=ALU.add), reads=[t_bis], writes=[t_mid])
                            for kc in range((nk + 1023) // 1024):
                                w = min(1024, nk - kc * 1024)
                                mq = kc % 2
                                sy.op("dve", lambda e, mq=mq, kc=kc, w=w: e.tensor_scalar(out=maskq[mq][:, 0:w], in0=isc[:, kc * 1024:kc * 1024 + w], scalar1=bis[:, 2:3], scalar2=None, op0=ALU.is_ge),
                                      reads=[t_isc, t_mid], writes=[t_mq[mq]])
                                k = kc % 2
                                for j in range(w // 128):
                                    sy.op("pe", lambda e, k=k, j=j, mq=mq: e.transpose(pb[k][:, j * 128:(j + 1) * 128], maskq[mq][:, j * 128:(j + 1) * 128], identb[:]),
                                          reads=[t_mq[mq], t_c], writes=[t_pb[k]])
                                sy.op("pool" if False else "dve", lambda e, k=k, kc=kc, w=w, jj=jj: e.tensor_scalar(out=maskbT[:, kc * 8:kc * 8 + w // 128, jj * 128:(jj + 1) * 128],
                                                                                          in0=pb[k][:, 0:w].rearrange("p (a t) -> p a t", t=128), scalar1=1.0, scalar2=None, op0=ALU.mult),
                                      reads=[t_pb[k]], writes=[t_mb[kc][jj]])
                            if pblk + 1 < NST:
                                c0 = (pblk + 1) // 8
                                sy.op("pool", lambda e, pblk=pblk, jj=jj: e.memset(maskbT[:, pblk + 1:NST, jj * 128:(jj + 1) * 128], 0.0),
                                      reads=[t_mb[c][jj] for c in range(c0, NST // 8)], writes=[t_mb[c][jj] for c in range(c0, NST // 8)])
                        sy.barrier()

                    with ExitStack() as st:
                        sy.enabled = "D" in PH
                        KTc = [sb(st, "KTc%d" % i, [128, 1024], BF16) for i in range(4)]
                        Vc = [sb(st, "Vc%d" % i, [128, 8, 128], BF16) for i in range(4)]
                        pT = [sb(st, "pT%d" % i, [128, 512], BF16) for i in range(6)]
                        rden = [sb(st, "rden%d" % i, [128, 512]) for i in range(2)]
                        ty = [sb(st, "ty%d" % i, [128, 512]) for i in range(2)]
                        t_KTc = toks(4); t_Vc = toks(4); t_pT = toks(6); t_rden = toks(2); t_ty = toks(2)
                        VV_v = VV_s.rearrange("(a p) n -> p a n", p=128)
                        li = 0
                        pti = 0
                        scale = 128.0 ** -0.5
                        NKC = NST // 8
                        tiles = [(h, kc, s_) for h in range(16) for kc in range(NKC) for s_ in range(8)]
                        chunk_buf = {}

                        def load_chunk(h, kc):
                            nonlocal_li = len(chunk_buf)
                            b = nonlocal_li % 4
                            chunk_buf[(h, kc)] = b
                            sy.dma(KTc[b][:], KT_s[h, :, kc * 1024:(kc + 1) * 1024], reads=[t_KT[kc][h]], writes=[t_KTc[b]])
                            sy.dma(Vc[b][:], VV_v[:, kc * 8:(kc + 1) * 8, h * 128:(h + 1) * 128], reads=t_VV[kc], writes=[t_Vc[b]])

                        def emit_logits(idx):
                            h, kc, s_ = tiles[idx]
                            if (h, kc) not in chunk_buf:
                                load_chunk(h, kc)
                                nxt = idx + 8 - (idx % 8)
                                if nxt < len(tiles) and (tiles[nxt][0], tiles[nxt][1]) not in chunk_buf:
                                    load_chunk(tiles[nxt][0], tiles[nxt][1])
                            b = chunk_buf[(h, kc)]
                            stg_ = kc * 8 + s_
                            pL, t_pL = pf[idx % 4], t_pf[idx % 4]
                            sy.op("pe", lambda e: e.matmul(pL[:], lhsT=KTc[b][:, s_ * 128:(s_ + 1) * 128], rhs=qT[:, h, :], start=True, stop=True),
                                  reads=[t_KTc[b], t_qT[h]], writes=[t_pL])

                        LA = 3
                        for i0 in range(min(LA, len(tiles))):
                            emit_logits(i0)
                        for idx, (h, kc, s_) in enumerate(tiles):
                            if idx + LA < len(tiles):
                                emit_logits(idx + LA)
                            b = chunk_buf[(h, kc)]
                            stg_ = kc * 8 + s_
                            pL, t_pL = pf[idx % 4], t_pf[idx % 4]
                            pO, t_pO = pf[4], t_pf[4]
                            pD, t_pD = pf[5], t_pf[5]
                            pi = idx % 6
                            sy.op("act", lambda e: e.activation(out=pT[pi][:], in_=pL[:], func=AF.Exp, scale=scale), reads=[t_pL], writes=[t_pT[pi]])
                            sy.op("dve", lambda e: e.tensor_tensor(out=pT[pi][:], in0=pT[pi][:], in1=maskbT[:, stg_, :], op=ALU.mult), reads=[t_pT[pi]] + t_mb[kc], writes=[t_pT[pi]])
                            sy.op("pe", lambda e: e.matmul(pO[:], lhsT=Vc[b][:, s_, :], rhs=pT[pi][:], start=(stg_ == 0), stop=(stg_ == NST - 1)),
                                  reads=[t_Vc[b], t_pT[pi]], writes=[t_pO])
                            sy.op("pe", lambda e: e.matmul(pD[:], lhsT=onesb[:], rhs=pT[pi][:], start=(stg_ == 0), stop=(stg_ == NST - 1)),
                                  reads=[t_c, t_pT[pi]], writes=[t_pD])
                            if stg_ == NST - 1:
                                b2 = h % 2
                                sy.op("dve", lambda e: e.reciprocal(out=rden[b2][:], in_=pD[:]), reads=[t_pD], writes=[t_rden[b2]])
                                sy.op("dve", lambda e: e.tensor_tensor(out=ty[b2][:], in0=pO[:], in1=rden[b2][:], op=ALU.mult), reads=[t_pO, t_rden[b2]], writes=[t_ty[b2]])
                                sy.op("pool", lambda e: e.tensor_tensor(out=ty[b2][:], in0=ty[b2][:], in1=sga[:, h, :], op=ALU.mult), reads=[t_ty[b2], t_sga[h]], writes=[t_ty[b2]])
                                sy.op("pool", lambda e: e.tensor_tensor(out=mixb[:, h, :], in0=ty[b2][:], in1=mixb[:, h, :], op=ALU.add), reads=[t_ty[b2], t_mixb[h]], writes=[t_mixb[h]])
                        sy.barrier()
                    sy.barrier()

                sy.enabled = "E" in PH
                ef = ExitStack()
                acc = sb(ef, "acc", [128, 4, D], side="right")
                xnT = sb(ef, "xnT", [128, 16, 512], BF16, side="right")
                t_acc = [[Tok() for _ in range(8)] for _ in range(4)]
                t_xnT = toks(4)
                with ExitStack() as st:
                    pf = [psum(st, "pfE%d" % i, [128, 512]) for i in range(6)]
                    t_pf = toks(6)
                    wring = [sb(st, "wringE%d" % i, [128, 16, 256], BF16) for i in range(4)]
                    t_wr = toks(4)
                    for ts in range(4):
                        r0 = (8 * g + 2 * ts + 1) * 128
                        sy.dma(acc[:, ts, :], xs[r0:r0 + 128, :], writes=t_acc[ts])
                    pfi = 0
                    for nb in range(8):
                        i = nb % 4
                        sy.dma(wring[i][:], Wo_v[:, :, nb * 256:(nb + 1) * 256], writes=[t_wr[i]])
                        for ts in range(4):
                            p_, t_p = pf[pfi % 6], t_pf[pfi % 6]
                            pfi += 1
                            for cc in range(16):
                                sy.op("pe", lambda e, p_=p_, cc=cc, ts=ts, i=i: e.matmul(p_[:, 0:256], lhsT=mixb[:, cc, ts * 128:(ts + 1) * 128], rhs=wring[i][:, cc, :], start=(cc == 0), stop=(cc == 15)),
                                      reads=[t_mixb[cc], t_wr[i]], writes=[t_p])
                            sy.op("dve", lambda e, p_=p_, ts=ts, nb=nb: e.tensor_tensor(out=acc[:, ts, nb * 256:(nb + 1) * 256], in0=p_[:, 0:256], in1=acc[:, ts, nb * 256:(nb + 1) * 256], op=ALU.add),
                                  reads=[t_p, t_acc[ts][nb]], writes=[t_acc[ts][nb]])
                    sy.barrier()
            sy.barrier()
            with ef:
                with ExitStack() as st:
                    pb = [psum(st, "pbE%d" % i, [128, 1024], BF16) for i in range(2)]
                    t_pb = toks(2)
                    gB = sb(st, "gBf", [128, D])
                    xnb = [sb(st, "xnb%d" % i, [128, D], BF16) for i in range(2)]
                    ssq = [sb(st, "ssqE%d" % i, [128, 1]) for i in range(2)]
                    t_g = Tok(); t_xnb = toks(2); t_ssq = toks(2)
                    sy.dma(gB[:], g_ffn_d.partition_broadcast(128), writes=[t_g])
                    for ts in range(4):
                        b = ts % 2
                        sy.op("act", lambda e, b=b, ts=ts: e.activation(out=xnb[b][:], in_=acc[:, ts, :], func=AF.Square, accum_out=ssq[b][:]), reads=t_acc[ts], writes=[t_xnb[b], t_ssq[b]])
                        rms_rstd(ssq[b][:], t_ssq[b], D)
                        sy.op("dve", lambda e, b=b, ts=ts: e.scalar_tensor_tensor(out=xnb[b][:], in0=acc[:, ts, :], scalar=ssq[b][:, 0:1], in1=gB[:], op0=ALU.mult, op1=ALU.mult),
                              reads=t_acc[ts] + [t_ssq[b], t_g], writes=[t_xnb[b]])
                        for k in range(2):
                            for j in range(8):
                                dk = k * 8 + j
                                sy.op("pe", lambda e, b=b, k=k, j=j, dk=dk: e.transpose(pb[k][:, j * 128:(j + 1) * 128], xnb[b][:, dk * 128:(dk + 1) * 128], identb[:]),
                                      reads=[t_xnb[b], t_c], writes=[t_pb[k]])
                            evac(xnT[:, k * 8:(k + 1) * 8, ts * 128:(ts + 1) * 128], pb[k][:].rearrange("p (a t) -> p a t", t=128), [t_pb[k]], [t_xnT[ts]])
                    sy.barrier()

                sy.enabled = "F" in PH or "f" in PH
                with ExitStack() as fs:
                    s1 = sb(fs, "s1", [128, 4, 8, 128])
                    s2 = sb(fs, "s2", [128, 4, 8, 128])
                    v1 = sb(fs, "v1", [128, 4, 8, 16])
                    v2 = sb(fs, "v2", [128, 4, 8, 16])
                    thra = sb(fs, "thra", [128, 4, 8])
                    E2t = sb(fs, "E2t", [128, 8, 128], BF16)
                    t_E2t = Tok()
                    rZ = sb(fs, "rZ", [128, 4, 8])
                    t_tab = toks(4)
                    pf = [psum(fs, "pfF%d" % i, [128, 512]) for i in range(8)]
                    t_pf = toks(8)
                    with ExitStack() as st:
                        qpT = sb(st, "qpT", [128, 16, 512], BF16)
                        t_qp = toks(16)
                        wring = [sb(st, "wringF%d" % i, [128, 16, 256], BF16) for i in range(4)]
                        t_wr = toks(4)
                        xw = sb(st, "xw", [128, 256])
                        cand = sb(st, "cand", [128, 8, 16, 16])
                        tops = sb(st, "tops", [128, 8, 16])
                        tmpz = sb(st, "tmpz", [128, 8, 16])
                        sm = sb(st, "sm", [128, 8, 4])
                        t_xw = Tok(); t_cand = Tok(); t_tops = Tok(); t_tmpz = Tok(); t_sm = Tok()
                        tmpE = sb(st, "tmpE", [128, 8, 128]); t_tmpE = Tok()
                        pfi = 0
                        for cp in range(8):
                            i = cp % 4
                            sy.dma(wring[i][:], Wq_v[:, :, cp * 256:(cp + 1) * 256], writes=[t_wr[i]])
                            for cc in range(2):
                                c = cp * 2 + cc
                                p_, t_p = pf[pfi % 4], t_pf[pfi % 4]
                                pfi += 1
                                for dk in range(16):
                                    sy.op("pe", lambda e, p_=p_, dk=dk, i=i, cc=cc: e.matmul(p_[:], lhsT=wring[i][:, dk, cc * 128:(cc + 1) * 128], rhs=xnT[:, dk, :], start=(dk == 0), stop=(dk == 15)),
                                          reads=[t_wr[i]] + t_xnT, writes=[t_p])
                                evac(qpT[:, c, :], p_[:], [t_p], [t_qp[c]])
                        for ts in range(4):
                            for bk in range(4):
                                p_, t_p = pf[4 + bk], t_pf[4 + bk]
                                for j in range(4):
                                    c = bk * 4 + j
                                    sy.op("pe", lambda e, p_=p_, j=j, c=c, ts=ts: e.matmul(p_[:, j * 128:(j + 1) * 128], lhsT=qpT[:, c, ts * 128:(ts + 1) * 128], rhs=(k1T if c % 2 == 0 else k2T)[:],
                                                                               start=True, stop=True), reads=[t_qp[c], t_c], writes=[t_p])
                                pv_ = p_[:].rearrange("p (h f k) -> p h f k", h=2, f=2)
                                sy.op("act", lambda e, pv_=pv_, ts=ts, bk=bk: e.activation(out=s1[:, ts, 2 * bk:2 * bk + 2, :], in_=pv_[:, :, 0, :], func=AF.Copy), reads=[t_p], writes=[t_tab[ts]])
                                sy.op("act", lambda e, pv_=pv_, ts=ts, bk=bk: e.activation(out=s2[:, ts, 2 * bk:2 * bk + 2, :], in_=pv_[:, :, 1, :], func=AF.Copy), reads=[t_p], writes=[t_tab[ts]])
                            for (sX, vX) in ((s1, v1), (s2, v2)):
                                for h in range(8):
                                    sy.op("dve", lambda e, sX=sX, vX=vX, ts=ts, h=h: e.max(out=vX[:, ts, h, 0:8], in_=sX[:, ts, h, :]), reads=[t_tab[ts]], writes=[t_tab[ts]])
                                    sy.op("dve", lambda e, sX=sX, vX=vX, ts=ts, h=h: e.match_replace(out=xw[:, 0:128], in_to_replace=vX[:, ts, h, 0:8], in_values=sX[:, ts, h, :], imm_value=NEG),
                                          reads=[t_tab[ts]], writes=[t_xw])
                                    sy.op("dve", lambda e, vX=vX, ts=ts, h=h: e.max(out=vX[:, ts, h, 8:16], in_=xw[:, 0:128]), reads=[t_xw], writes=[t_tab[ts]])
                            sy.op("dve", lambda e, ts=ts: e.tensor_tensor(out=cand[:], in0=v1[:, ts, :, :].unsqueeze(3).to_broadcast([128, 8, 16, 16]),
                                                                    in1=v2[:, ts, :, :].unsqueeze(2).to_broadcast([128, 8, 16, 16]), op=ALU.add), reads=[t_tab[ts]], writes=[t_cand])
                            for h in range(8):
                                ch = cand[:, h, :, :].rearrange("p a b -> p (a b)")
                                sy.op("dve", lambda e, ch=ch, h=h: e.max(out=tops[:, h, 0:8], in_=ch), reads=[t_cand], writes=[t_tops])
                                sy.op("dve", lambda e, ch=ch, h=h: e.match_replace(out=xw[:], in_to_replace=tops[:, h, 0:8], in_values=ch, imm_value=NEG), reads=[t_cand, t_tops], writes=[t_xw])
                                sy.op("dve", lambda e, h=h: e.max(out=tops[:, h, 8:16], in_=xw[:]), reads=[t_xw], writes=[t_tops])
                            sy.op("dve", lambda e: e.tensor_tensor(out=tmpz[:], in0=tops[:], in1=tops[:, :, 0:1].to_broadcast([128, 8, 16]), op=ALU.subtract), reads=[t_tops], writes=[t_tmpz])
                            sy.op("act", lambda e: e.activation(out=tmpz[:], in_=tmpz[:], func=AF.Exp), reads=[t_tmpz], writes=[t_tmpz])
                            sy.op("dve", lambda e: e.tensor_reduce(out=sm[:, :, 0], in_=tmpz[:], axis=AX.X, op=ALU.add), reads=[t_tmpz], writes=[t_sm])
                            sy.op("act", lambda e: e.activation(out=sm[:, :, 2], in_=sm[:, :, 0], func=AF.Ln), reads=[t_sm], writes=[t_sm])
                            sy.op("dve", lambda e, ts=ts: e.tensor_tensor(out=rZ[:, ts, :], in0=sm[:, :, 2], in1=tops[:, :, 0], op=ALU.add), reads=[t_sm, t_tops], writes=[t_tab[ts]])
                            sy.op("act", lambda e: e.activation(out=sm[:, :, 1], in_=tops[:, :, 0], func=AF.Abs, scale=PEER_MARGIN), reads=[t_tops, t_sm], writes=[t_sm])
                            sy.op("dve", lambda e, ts=ts: e.tensor_tensor(out=thra[:, ts, :], in0=tops[:, :, 15], in1=sm[:, :, 1], op=ALU.subtract), reads=[t_tops, t_sm], writes=[t_tab[ts]])
                            if ts == 3:
                                sy.op("dve", lambda e: e.tensor_tensor(out=tmpE[:], in0=s2[:, 3, :, :], in1=v2[:, 3, :, 0:1].to_broadcast([128, 8, 128]), op=ALU.subtract), reads=[t_tab[3]], writes=[t_tmpE])
                                sy.op("act", lambda e: e.activation(out=E2t[:], in_=tmpE[:], func=AF.Exp), reads=[t_tmpE], writes=[t_E2t])
                        sy.barrier()

                    sy.enabled = "F" in PH
                    with ExitStack() as st:
                        uTb = [sb(st, "uTb%d" % i, [128, 16, 128], BF16) for i in range(4)]
                        vb = [sb(st, "vb%d" % i, [128, SG, 512], BF16) for i in range(2)]
                        actG = [sb(st, "actG%d" % i, [128, SG, 512], BF16) for i in range(2)]
                        gz = [sb(st, "gz%d" % i, [128, 512]) for i in range(2)]
                        Ab = [sb(st, "Ab%d" % i, [128, 16, 128], BF16) for i in range(4)]
                        Ew = [sb(st, "Ew%d" % i, [128, 32, 128], BF16) for i in range(2)]
                        t_Ew = [toks(32) for _ in range(2)]
                        thS = sb(st, "thS", [128, 2, 4, 8, SG])
                        w1S = sb(st, "w1S", [128, 2, 4, 8, SG])
                        w13S = sb(st, "w13S", [128, 2, 8, SG])
                        t_uT = toks(4); t_vb = toks(2); t_gz = toks(2); t_Ab = toks(4)
                        t_actG = [[Tok() for _ in range(SG)] for _ in range(2)]
                        t_thS = toks(2)
                        NSG = 128 // SG

                        def sg_tables(sg_):
                            sb_ = sg_ % 2
                            i1s = slice(sg_ * SG, (sg_ + 1) * SG)
                            for ts in range(4):
                                sy.op("dve", lambda e, ts=ts: e.tensor_tensor(out=thS[:, sb_, ts, :, :], in0=thra[:, ts, :].unsqueeze(2).to_broadcast([128, 8, SG]), in1=s1[:, ts, :, i1s], op=ALU.subtract),
                                      reads=[t_tab[ts]], writes=[t_thS[sb_]])
                                sy.op("dve", lambda e, ts=ts: e.tensor_tensor(out=w1S[:, sb_, ts, :, :], in0=s1[:, ts, :, i1s], in1=rZ[:, ts, :].unsqueeze(2).to_broadcast([128, 8, SG]), op=ALU.subtract),
                                      reads=[t_tab[ts]], writes=[t_thS[sb_]])
                            sy.op("dve", lambda e: e.tensor_tensor(out=w13S[:, sb_, :, :], in0=w1S[:, sb_, 3, :, :], in1=v2[:, 3, :, 0:1].to_broadcast([128, 8, SG]), op=ALU.add),
                                  reads=[t_tab[3], t_thS[sb_]], writes=[t_thS[sb_]])
                            sy.op("act", lambda e: e.activation(out=w13S[:, sb_, :, :].rearrange("p a b -> p (a b)"), in_=w13S[:, sb_, :, :].rearrange("p a b -> p (a b)"), func=AF.Exp), reads=[t_thS[sb_]], writes=[t_thS[sb_]])

                        def st_exp(i1):
                            sg_, il = divmod(i1, SG)
                            sb_ = sg_ % 2
                            if il == 0:
                                sg_tables(sg_)
                            ub = i1 % 4
                            sy.dma(uTb[ub][:].rearrange("p a b -> p (a b)"), UT_bf[i1, :, :], writes=[t_uT[ub]])
                            ei = i1 % 2
                            for ts in range(3):
                                for h in range(8):
                                    sy.op("act", lambda e, ts=ts, h=h: e.activation(out=Ew[ei][:, ts * 8 + h, :], in_=s2[:, ts, h, :], func=AF.Exp, bias=w1S[:, sb_, ts, h, il:il + 1], scale=1.0),
                                          reads=[t_tab[ts], t_thS[sb_]], writes=[t_Ew[ei][ts * 8 + h]])
                            sy.op("dve", lambda e: e.tensor_tensor(out=Ew[ei][:, 24:32, :], in0=E2t[:], in1=w13S[:, sb_, :, il:il + 1].to_broadcast([128, 8, 128]), op=ALU.mult),
                                  reads=[t_E2t, t_thS[sb_]], writes=t_Ew[ei][24:32])

                        def st_m(i1):
                            sg_, il = divmod(i1, SG)
                            sb_ = sg_ % 2
                            ei = i1 % 2
                            for tp in range(2):
                                ai = (i1 % 2) * 2 + tp
                                A = Ab[ai]
                                tsl = slice(2 * tp, 2 * tp + 2)
                                rd = [t_tab[2 * tp], t_tab[2 * tp + 1]]
                                sy.op("dve", lambda e, A=A, tsl=tsl: e.tensor_tensor(out=A[:], in0=s2[:, tsl, :, :].rearrange("p a h i -> p (a h) i"),
                                                                                  in1=thS[:, sb_, tsl, :, il:il + 1].rearrange("p a h o -> p (a h) o").to_broadcast([128, 16, 128]), op=ALU.is_ge),
                                      reads=rd + [t_thS[sb_]], writes=[t_Ab[ai]])
                                sy.op("dve", lambda e, A=A, tp=tp: e.tensor_tensor(out=A[:], in0=A[:], in1=Ew[ei][:, tp * 16:(tp + 1) * 16, :], op=ALU.mult),
                                      reads=[t_Ab[ai]] + t_Ew[ei][tp * 16:(tp + 1) * 16], writes=[t_Ab[ai]])

                        def st_z(i1):
                            ub = i1 % 4
                            pz, t_pz = pf[i1 % 2], t_pf[i1 % 2]
                            for dk in range(16):
                                sy.op("pe", lambda e, dk=dk: e.matmul(pz[:], lhsT=uTb[ub][:, dk, :], rhs=xnT[:, dk, :], start=(dk == 0), stop=(dk == 15)),
                                      reads=[t_uT[ub]] + t_xnT, writes=[t_pz])

                        def st_G(i1):
                            pG, t_pG = pf[2 + i1 % 2], t_pf[2 + i1 % 2]
                            for tp in range(2):
                                ai = (i1 % 2) * 2 + tp
                                A = Ab[ai]
                                for tl in range(2):
                                    ts = 2 * tp + tl
                                    for h in range(8):
                                        sy.op("pe", lambda e, A=A, tl=tl, h=h, ts=ts: e.matmul(pG[:, ts * 128:(ts + 1) * 128], lhsT=A[:, tl * 8 + h, :], rhs=identb[:], start=(h == 0), stop=(h == 7)),
                                              reads=[t_Ab[ai], t_c], writes=[t_pG])

                        def st_gelu(i1):
                            sg_, il = divmod(i1, SG)
                            sb_ = sg_ % 2
                            pG, t_pG = pf[2 + i1 % 2], t_pf[2 + i1 % 2]
                            pz, t_pz = pf[i1 % 2], t_pf[i1 % 2]
                            zb = i1 % 2
                            sy.op("act", lambda e: e.activation(out=gz[zb][:], in_=pz[:], func=AF.Gelu), reads=[t_pz], writes=[t_gz[zb]])
                            sy.op("dve", lambda e: e.tensor_tensor(out=actG[sb_][:, il, :], in0=gz[zb][:], in1=pG[:], op=ALU.mult), reads=[t_gz[zb], t_pG], writes=[t_actG[sb_][il]])
                            if il == SG - 1:
                                for nb in range(4):
                                    f2_queue.append((sg_, nb))

                        f2_queue = []

                        def f2_slice():
                            if not f2_queue:
                                return
                            sg_, nb = f2_queue.pop(0)
                            sb_ = sg_ % 2
                            i1s = slice(sg_ * SG, (sg_ + 1) * SG)
                            vbi = nb % 2
                            sy.dma(vb[vbi][:], PV_v[:, i1s, nb * 512:(nb + 1) * 512], writes=[t_vb[vbi]])
                            for ts in range(4):
                                pO, t_pO = pf[4 + ts], t_pf[4 + ts]
                                for il2 in range(SG):
                                    sy.op("pe", lambda e, il2=il2: e.matmul(pO[:], lhsT=actG[sb_][:, il2, ts * 128:(ts + 1) * 128], rhs=vb[vbi][:, il2, :], start=(il2 == 0), stop=(il2 == SG - 1)),
                                          reads=[t_actG[sb_][il2], t_vb[vbi]], writes=[t_pO])
                                sy.op("dve", lambda e: e.tensor_tensor(out=acc[:, ts, nb * 512:(nb + 1) * 512], in0=pO[:], in1=acc[:, ts, nb * 512:(nb + 1) * 512], op=ALU.add),
                                      reads=[t_pO, t_acc[ts][2 * nb], t_acc[ts][2 * nb + 1]], writes=[t_acc[ts][2 * nb], t_acc[ts][2 * nb + 1]])

                        for i1 in (0, 1):
                            st_exp(i1)
                        for i1 in (0, 1):
                            st_m(i1)
                        for i1 in (0, 1):
                            st_z(i1)
                        for s_ in range(0, 128, 2):
                            st_G(s_)
                            if s_ + 2 < 128:
                                st_exp(s_ + 2)
                                st_m(s_ + 2)
                                st_exp(s_ + 3)
                            st_gelu(s_)
                            st_G(s_ + 1)
                            if s_ + 3 < 128:
                                st_m(s_ + 3)
                            st_gelu(s_ + 1)
                            if s_ + 2 < 128:
                                st_z(s_ + 2)
                                st_z(s_ + 3)
                            f2_slice()
                        while f2_queue:
                            f2_slice()
                        sy.barrier()
                    sy.barrier()

                sy.enabled = "G" in PH
                with ExitStack() as st:
                    gB = sb(st, "gBz", [128, D])
                    ot = [sb(st, "ot%d" % i, [128, D]) for i in range(2)]
                    ssq = [sb(st, "ssqG%d" % i, [128, 1]) for i in range(2)]
                    t_g = Tok(); t_ot = toks(2); t_ssq = toks(2)
                    sy.dma(gB[:], g_fin_d.partition_broadcast(128), writes=[t_g])
                    for ts in range(4):
                        b = ts % 2
                        sy.op("act", lambda e, b=b, ts=ts: e.activation(out=ot[b][:], in_=acc[:, ts, :], func=AF.Square, accum_out=ssq[b][:]), reads=t_acc[ts], writes=[t_ot[b], t_ssq[b]])
                        rms_rstd(ssq[b][:], t_ssq[b], D)
                        sy.op("dve", lambda e, b=b, ts=ts: e.scalar_tensor_tensor(out=ot[b][:], in0=acc[:, ts, :], scalar=ssq[b][:, 0:1], in1=gB[:], op0=ALU.mult, op1=ALU.mult),
                              reads=t_acc[ts] + [t_ssq[b], t_g], writes=[t_ot[b]])
                        r0 = (4 * g + ts) * 128
                        sy.dma(y_d[r0:r0 + 128, :], ot[b][:], reads=[t_ot[b]])
                    sy.barrier()
            sy.barrier()
        sy.finish()
        build_program.ninstr = sy.ninstr
    return nc


_CACHE = {}


def prep_inputs(inp, S):
    f = lambda a: np.ascontiguousarray(np.asarray(a, dtype=np.float32))
    x = f(inp["x"])
    B = x.shape[0]
    q = np.arange(128)
    tri = np.where(q[None, :] <= q[:, None], 0.0, NEG).astype(np.float32)
    bw = 128
    rnnp = np.stack([f(inp["conv_w"])[0, 0], f(inp["conv_w"])[0, 1], f(inp["conv_w"])[0, 2], f(inp["conv_w"])[0, 3],
                     f(inp["conv_b"])[0], f(inp["rg_ba"])[0], f(inp["rg_bx"])[0], f(inp["rg_lambda"])[0]], axis=0)
    rnnp = np.ascontiguousarray(rnnp.reshape(8, 16, 128).transpose(2, 0, 1))
    common = {
        "tri": tri,
        "rnnp": rnnp,
        "g_mix": f(inp["norm_mix_g"])[0], "g_ffn": f(inp["norm_ffn_g"])[0], "g_fin": f(inp["norm_final_g"]),
        "g_kv": f(inp["kv_norm_g"])[0], "idx_g": f(inp["idx_ln_g"])[0], "idx_b": f(inp["idx_ln_b"])[0],
        "w_in": f(inp["w_in"])[0], "w_o": f(inp["w_o"])[0], "wq": f(inp["peer_wq"])[0],
        "wuk": np.ascontiguousarray(f(inp["w_uk"])[0].reshape(16, 4, 128, 128).transpose(0, 2, 1, 3)),
        "wuv": np.ascontiguousarray(f(inp["w_uv"])[0].transpose(1, 0, 2).reshape(512, D)),
        "rgwa": np.ascontiguousarray(f(inp["rg_wa"])[0].transpose(1, 0, 2)),
        "rgwx": np.ascontiguousarray(f(inp["rg_wx"])[0].transpose(1, 0, 2)),
        "k1T": np.ascontiguousarray(f(inp["peer_keys1"])[0].T), "k2T": np.ascontiguousarray(f(inp["peer_keys2"])[0].T),
        "uT": np.ascontiguousarray(f(inp["peer_u"])[0].reshape(128, 128, 16, 128).transpose(0, 3, 2, 1)).reshape(128, 128, 2048),
        "pv": f(inp["peer_v"])[0],
        "pow2": np.ascontiguousarray(np.broadcast_to((0.5 ** np.arange(1, NBIS + 2)).astype(np.float32), (128, NBIS + 1))),
    }
    maps = []
    for b in range(B):
        for hh in range(2):
            m = dict(common)
            if hh == 1:
                m["xs"] = x[b]
                m["kb0"] = np.zeros((128, 128), np.float32)
                m["vrow"] = np.ones((128, 128), np.float32)
            else:
                xs = np.zeros((S, D), np.float32)
                xs[128:] = x[b, :S - 128]
                m["xs"] = xs
                m["kb0"] = np.full((128, 128), NEG, np.float32)
                m["vrow"] = np.zeros((128, 128), np.float32)
            maps.append(m)
    return maps


def assemble(results, B, S):
    out = np.empty((B, S, D), np.float32)
    ov = out.reshape(B, S // 256, 2, 128, D)
    for b in range(B):
        for hh in range(2):
            y = np.asarray(results[b * 2 + hh]["y"]).reshape(S // 256, 128, D)
            ov[b, :, hh] = y
    return out


def kernel(**inputs):
    x = np.asarray(inputs["x"])
    B, S, _ = x.shape
    if S not in _CACHE:
        _CACHE[S] = build_program(S)
    nc = _CACHE[S]
    maps = prep_inputs(inputs, S)
    res = run_bass_kernel_spmd(nc, maps, core_ids=list(range(2 * B)))
    return assemble(res.results, B, S)
```

```python
import numpy as np
from contextlib import ExitStack
import concourse.bass as bass
import concourse.mybir as mybir
from concourse.bass_utils import run_bass_kernel_spmd

F32 = mybir.dt.float32
BF16 = mybir.dt.bfloat16
FP8 = mybir.dt.float8e4
AF = mybir.ActivationFunctionType
ALU = mybir.AluOpType
AX = mybir.AxisListType

D = 2048
NDK = 16
IN_W = 9808
C_XR, C_Q, C_CKV, C_QI, C_KI, C_GR, C_GA = 0, 2048, 4096, 4608, 5632, 5712, 7760
EPS = 1e-6
NEG = -1.0e30
BIGM = 30000.0
NBIS = 18
PEER_MARGIN = 4e-6
SG = 8


class Tok:
    __slots__ = ("w", "r")

    def __init__(self):
        self.w = None
        self.r = []


def toks(n):
    return [Tok() for _ in range(n)]


class Sync:
    NDS = 24

    def __init__(self, nc, es):
        self.nc = nc
        self.eng = {"pe": nc.tensor, "dve": nc.vector, "act": nc.scalar, "pool": nc.gpsimd, "sp": nc.sync}
        self.sem = {}
        self.cnt = {}
        for k in ["pe", "dve", "act", "pool"]:
            self.sem[k] = es.enter_context(nc.semaphore("sem_" + k))
            self.cnt[k] = 0
        for i in range(self.NDS):
            self.sem[("d", i)] = es.enter_context(nc.semaphore("dsem%d" % i))
            self.cnt[("d", i)] = 0
        self.dnext = 0
        self.waited = {k: {} for k in self.eng}
        self.ninstr = 0
        self.enabled = True

    def _wait(self, e, key, val):
        if self.waited[e].get(key, 0) >= val:
            return
        self.eng[e].wait_ge(self.sem[key], val)
        self.waited[e][key] = val

    def _deps(self, e, reads, writes):
        deps = {}
        for t in reads:
            if t.w is not None:
                k, v = t.w
                if deps.get(k, 0) < v:
                    deps[k] = v
        for t in writes:
            if t.w is not None:
                k, v = t.w
                if deps.get(k, 0) < v:
                    deps[k] = v
            for k, v in t.r:
                if deps.get(k, 0) < v:
                    deps[k] = v
        for k, v in deps.items():
            if e == "pe" and k == "pe":
                continue
            self._wait(e, k, v)

    def _commit(self, ev, reads, writes):
        for t in reads:
            t.r.append(ev)
            if len(t.r) > 48:
                best = {}
                for k, v in t.r:
                    if best.get(k, 0) < v:
                        best[k] = v
                t.r = list(best.items())
        for t in writes:
            t.w = ev
            t.r = []

    def op(self, e, fn, reads=(), writes=()):
        if not self.enabled:
            return None
        self._deps(e, reads, writes)
        ins = fn(self.eng[e])
        self.cnt[e] += 1
        ins.then_inc(self.sem[e], 1)
        ev = (e, self.cnt[e])
        self._commit(ev, reads, writes)
        self.ninstr += 1
        return ev

    def dma(self, out, in_, reads=(), writes=(), q="sp"):
        if not self.enabled:
            return None
        slot = self.dnext % self.NDS
        self.dnext += 1
        key = ("d", slot)
        if self.cnt[key] > 0:
            self._wait(q, key, self.cnt[key])
        self._deps(q, reads, writes)
        ins = self.eng[q].dma_start(out=out, in_=in_)
        self.cnt[key] += 16
        ins.then_inc(self.sem[key], 16)
        ev = (key, self.cnt[key])
        self._commit(ev, reads, writes)
        self.ninstr += 1
        return ev

    def barrier(self):
        for e in self.eng:
            for k, v in self.cnt.items():
                if v > 0:
                    self._wait(e, k, v)

    def finish(self, e="sp"):
        for k, v in self.cnt.items():
            if v > 0:
                self._wait(e, k, v)


PH = "PABCDEFG"


def build_program(S, dbg=False):
    NG = S // 1024
    NBLK = S // 128
    NOWN = NBLK // 2
    TOPK = float(min(256, S // 4))
    nc = bass.Bass("TRN2", target_bir_lowering=False)

    def din(name, shape, dt=F32):
        return nc.dram_tensor(name, list(shape), dt, kind="ExternalInput").ap()

    def dscr(name, shape, dt=BF16):
        return nc.dram_tensor(name, list(shape), dt, kind="Internal").ap()

    xs = din("xs", [S, D])
    tri_d = din("tri", [128, 128])
    kb0_d = din("kb0", [128, 128])
    vrow_d = din("vrow", [128, 128])
    rnnp_d = din("rnnp", [128, 8, 16])
    g_mix_d = din("g_mix", [D])
    g_ffn_d = din("g_ffn", [D])
    g_fin_d = din("g_fin", [D])
    g_kv_d = din("g_kv", [512])
    idx_g_d = din("idx_g", [64])
    idx_b_d = din("idx_b", [64])
    w_in_d = din("w_in", [D, IN_W])
    w_o_d = din("w_o", [D, D])
    wq_d = din("wq", [D, D])
    wuk_d = din("wuk", [16, 128, 4, 128])
    wuv_d = din("wuv", [512, D])
    rgwa_d = din("rgwa", [128, 16, 128])
    rgwx_d = din("rgwx", [128, 16, 128])
    k1T_d = din("k1T", [128, 128])
    k2T_d = din("k2T", [128, 128])
    uT_d = din("uT", [128, 128, 16 * 128])
    pv_d = din("pv", [16384, D])
    pow2_d = din("pow2", [128, NBIS + 1])
    y_d = nc.dram_tensor("y", [NOWN * 128, D], F32, kind="ExternalOutput").ap()

    Win_bf = dscr("Win_bf", [D, IN_W])
    Wo_bf = dscr("Wo_bf", [D, D])
    Wq_bf = dscr("Wq_bf", [D, D])
    Wuk_bf = dscr("Wuk_bf", [16, 128, 512])
    Wuv_bf = dscr("Wuv_bf", [512, D])
    UT_bf = dscr("UT_bf", [128, 128, 2048])
    PV_bf = dscr("PV_bf", [16384, D])
    KT_s = dscr("KT_s", [16, 128, S])
    VV_s = dscr("VV_s", [S, D])

    es = ExitStack()
    with es:
        sy = Sync(nc, es)

        uid = {"n": 0}

        def sb(st, name, shape, dt=F32, side=None):
            uid["n"] += 1
            name = "s%d_%s" % (uid["n"], name)
            if side is None:
                return st.enter_context(nc.sbuf_tensor(name, list(shape), dt))
            return st.enter_context(nc.sbuf_tensor(name, list(shape), dt, side=side))

        def psum(st, name, shape, dt=F32):
            uid["n"] += 1
            name = "p%d_%s" % (uid["n"], name)
            return st.enter_context(nc.psum_tensor(name, list(shape), dt))

        rr = {"n": 0}

        def evac(out, in_, reads, writes, engines=("act", "dve")):
            e = engines[rr["n"] % len(engines)]
            rr["n"] += 1
            if e == "act":
                return sy.op("act", lambda en: en.activation(out=out, in_=in_, func=AF.Copy), reads, writes)
            return sy.op(e, lambda en: en.tensor_copy(out=out, in_=in_), reads, writes)

        identf = sb(es, "identf", [128, 128])
        identb = sb(es, "identb", [128, 128], BF16)
        onesb = sb(es, "onesb", [128, 128], BF16)
        tri = sb(es, "tri_s", [128, 128])
        kb0 = sb(es, "kb0_s", [128, 128])
        vrow = sb(es, "vrow_s", [128, 128])
        rnnp = sb(es, "rnnp_s", [128, 8, 16])
        cA = sb(es, "cA", [128, 16])
        cA2 = sb(es, "cA2", [128, 16])
        rgwa = sb(es, "rgwa_s", [128, 16, 128], BF16)
        rgwx = sb(es, "rgwx_s", [128, 16, 128], BF16)
        k1T = sb(es, "k1T_s", [128, 128], BF16)
        k2T = sb(es, "k2T_s", [128, 128], BF16)
        gkvB = sb(es, "gkvB", [128, 512])
        idxgB = sb(es, "idxgB", [128, 64])
        idxbB = sb(es, "idxbB", [128, 64])
        halo = sb(es, "halo", [128, 16, 3])
        hstate = sb(es, "hstate", [128, 16])
        pow2 = sb(es, "pow2_s", [128, NBIS + 1])
        epsT = sb(es, "epsT", [128, 1])
        oneT = sb(es, "oneT", [128, 1])
        kiT2 = sb(es, "kiT2", [128, S], BF16)
        t_c = Tok()
        t_halo = toks(16)
        t_hst = toks(16)
        t_kiT = toks(NBLK)

        with ExitStack() as st:
            stg = [sb(st, "cv_in%d" % i, [128, 2048]) for i in range(3)]
            cvo = [sb(st, "cv_out%d" % i, [128, 2048], BF16) for i in range(3)]
            t_in = toks(3)
            t_out = toks(3)
            CW = 4096
            NCB = 4
            stg2 = [sb(st, "cv2_in%d" % i, [128, CW]) for i in range(NCB)]
            cvo2 = [sb(st, "cv2_out%d" % i, [128, CW], BF16) for i in range(NCB)]
            t_in2 = toks(NCB)
            t_out2 = toks(NCB)
            sy.op("pool", lambda e: e.memset(identf[:], 1.0), writes=[t_c])
            sy.op("pool", lambda e: e.affine_select(out=identf[:], in_=identf[:], pattern=[[-1, 128]],
                                                    compare_op=ALU.is_equal, fill=0.0, base=0,
                                                    channel_multiplier=1), reads=[t_c], writes=[t_c])
            sy.op("pool", lambda e: e.tensor_copy(out=identb[:], in_=identf[:]), reads=[t_c], writes=[t_c])
            sy.op("pool", lambda e: e.memset(onesb[:], 1.0), writes=[t_c])
            sy.op("pool", lambda e: e.memset(epsT[:], EPS), writes=[t_c])
            sy.op("pool", lambda e: e.memset(oneT[:], 1.0), writes=[t_c])
            sy.op("pool", lambda e: e.memset(halo[:], 0.0), writes=t_halo)
            sy.op("pool", lambda e: e.memset(hstate[:], 0.0), writes=t_hst)
            tl = toks(12)
            sy.dma(tri[:], tri_d[:, :], writes=[tl[0]])
            sy.dma(kb0[:], kb0_d[:, :], writes=[tl[1]])
            sy.dma(vrow[:], vrow_d[:, :], writes=[tl[2]])
            sy.dma(rnnp[:], rnnp_d[:, :, :], writes=[tl[3]])
            sy.dma(gkvB[:], g_kv_d.partition_broadcast(128), writes=[tl[4]])
            sy.dma(idxgB[:], idx_g_d.partition_broadcast(128), writes=[tl[5]])
            sy.dma(idxbB[:], idx_b_d.partition_broadcast(128), writes=[tl[6]])
            sy.dma(pow2[:], pow2_d[:, :], writes=[tl[7]])
            for i, (src, dst) in enumerate([(rgwa_d, rgwa), (rgwx_d, rgwx)]):
                sy.dma(stg[i][:, :].rearrange("p (a b) -> p a b", b=128), src[:, :, :], writes=[t_in[i]])
                sy.op("dve", lambda e, i=i, dst=dst: e.tensor_copy(out=dst[:].rearrange("p a b -> p (a b)"), in_=stg[i][:, :]),
                      reads=[t_in[i]], writes=[t_c])
            sy.dma(stg[2][:, 0:128], k1T_d[:, :], writes=[t_in[2]])
            sy.dma(stg[2][:, 128:256], k2T_d[:, :], writes=[t_in[2]])
            sy.op("dve", lambda e: e.tensor_copy(out=k1T[:], in_=stg[2][:, 0:128]), reads=[t_in[2]], writes=[t_c])
            sy.op("dve", lambda e: e.tensor_copy(out=k2T[:], in_=stg[2][:, 128:256]), reads=[t_in[2]], writes=[t_c])
            sy.op("act", lambda e: e.activation(out=cA[:], in_=rnnp[:, 7, :], func=AF.Exp, scale=-1.0), reads=[tl[3]], writes=[t_c])
            sy.op("act", lambda e: e.activation(out=cA[:], in_=cA[:], func=AF.Ln, bias=oneT[:], scale=1.0), reads=[t_c], writes=[t_c])
            sy.op("dve", lambda e: e.tensor_scalar(out=cA2[:], in0=cA[:], scalar1=-16.0, scalar2=None, op0=ALU.mult), reads=[t_c], writes=[t_c])
            sy.op("dve", lambda e: e.tensor_scalar(out=cA[:], in0=cA[:], scalar1=-8.0, scalar2=None, op0=ALU.mult), reads=[t_c], writes=[t_c])
            sy.barrier()

            pieces = []

            def conv_flat(src, dst, numel):
                L = numel // 128
                sv = src.rearrange("(p l) -> p l", p=128)
                dv = dst.rearrange("(p l) -> p l", p=128)
                for c0 in range(0, L, CW):
                    pieces.append((sv, dv, c0, min(CW, L - c0)))
            conv_flat(w_in_d.rearrange("a b -> (a b)"), Win_bf.rearrange("a b -> (a b)"), D * IN_W)
            conv_flat(w_o_d.rearrange("a b -> (a b)"), Wo_bf.rearrange("a b -> (a b)"), D * D)
            conv_flat(wq_d.rearrange("a b -> (a b)"), Wq_bf.rearrange("a b -> (a b)"), D * D)
            conv_flat(wuk_d.rearrange("a b c d -> (a b c d)"), Wuk_bf.rearrange("a b c -> (a b c)"), 16 * 128 * 512)
            conv_flat(wuv_d.rearrange("a b -> (a b)"), Wuv_bf.rearrange("a b -> (a b)"), 512 * D)
            conv_flat(uT_d.rearrange("a b c -> (a b c)"), UT_bf.rearrange("a b c -> (a b c)"), 16384 * D)
            conv_flat(pv_d.rearrange("a b -> (a b)"), PV_bf.rearrange("a b -> (a b)"), 16384 * D)

            def cv_load(k):
                sv, dv, c0, w = pieces[k]
                b = k % NCB
                sy.dma(stg2[b][:, 0:w], sv[:, c0:c0 + w], writes=[t_in2[b]])
            for k in range(min(2, len(pieces))):
                cv_load(k)
            for k, (sv, dv, c0, w) in enumerate(pieces):
                if k + 2 < len(pieces):
                    cv_load(k + 2)
                b = k % NCB
                if k % 2 == 0:
                    sy.op("act", lambda e, b=b, w=w: e.activation(out=cvo2[b][:, 0:w], in_=stg2[b][:, 0:w], func=AF.Copy), reads=[t_in2[b]], writes=[t_out2[b]])
                else:
                    sy.op("dve", lambda e, b=b, w=w: e.tensor_copy(out=cvo2[b][:, 0:w], in_=stg2[b][:, 0:w]), reads=[t_in2[b]], writes=[t_out2[b]])
                sy.dma(dv[:, c0:c0 + w], cvo2[b][:, 0:w], reads=[t_out2[b]])
            sy.barrier()

        Win_v = Win_bf.rearrange("(dk p) c -> p dk c", p=128)
        Wo_v = Wo_bf.rearrange("(dk p) c -> p dk c", p=128)
        Wq_v = Wq_bf.rearrange("(dk p) c -> p dk c", p=128)
        Wuv_v = Wuv_bf.rearrange("(cc p) n -> p cc n", p=128)
        PV_v = PV_bf.rearrange("(i p) n -> p i n", p=128)

        t_KT = [[Tok() for _ in range(16)] for _ in range(NG)]
        t_VV = [[Tok() for _ in range(8)] for _ in range(NG)]

        def rms_rstd(st_ssq, t_ssq, n_feat):
            sy.op("act", lambda e: e.activation(out=st_ssq, in_=st_ssq, func=AF.Sqrt, bias=epsT[:], scale=1.0 / n_feat),
                  reads=[t_ssq, t_c], writes=[t_ssq])
            sy.op("dve", lambda e: e.reciprocal(out=st_ssq, in_=st_ssq), reads=[t_ssq], writes=[t_ssq])

        for g in range(NG):
            with ExitStack() as mix:
                qT = sb(mix, "qT", [128, 16, 512], BF16)
                qiT = sb(mix, "qiT", [128, 8, 512], BF16)
                sga = sb(mix, "sga", [128, 16, 512], BF16)
                mixb = sb(mix, "mixb", [128, 16, 512], BF16)
                absw = sb(mix, "absw", [128, 4, 16])
                sgnw = sb(mix, "sgnw", [128, 4, 16])
                t_qT = toks(16)
                t_qiT = toks(8)
                t_sga = toks(16)
                t_mixb = toks(16)
                t_w = toks(4)
                with ExitStack() as ab:
                    hT = sb(ab, "hT", [128, 16, 1024], BF16)
                    t_hT = toks(8)
                    hTo = hT[:].rearrange("p k (a two t) -> p k a two t", two=2, t=128)
                    pf = [psum(ab, "pfA%d" % i, [128, 512]) for i in range(6)]
                    pb = [psum(ab, "pbA%d" % i, [128, 1024], BF16) for i in range(2)]
                    t_pf = toks(6)
                    t_pb = toks(2)
                    sy.enabled = "A" in PH
                    with ExitStack() as st:
                        xt = [sb(st, "xt%d" % i, [128, D]) for i in range(2)]
                        hb = [sb(st, "hb%d" % i, [128, D], BF16) for i in range(2)]
                        ssq = [sb(st, "ssq%d" % i, [128, 1]) for i in range(2)]
                        gB = sb(st, "gB", [128, D])
                        t_x = toks(2); t_hb = toks(2); t_ssq = toks(2); t_g = Tok()
                        sy.dma(gB[:], g_mix_d.partition_broadcast(128), writes=[t_g])
                        for tt in range(8):
                            b = tt % 2
                            r0 = g * 1024 + tt * 128
                            sy.dma(xt[b][:], xs[r0:r0 + 128, :], writes=[t_x[b]])
                            sy.op("act", lambda e, b=b: e.activation(out=hb[b][:], in_=xt[b][:], func=AF.Square, accum_out=ssq[b][:]),
                                  reads=[t_x[b]], writes=[t_hb[b], t_ssq[b]])
                            rms_rstd(ssq[b][:], t_ssq[b], D)
                            sy.op("dve", lambda e, b=b: e.scalar_tensor_tensor(out=hb[b][:], in0=xt[b][:], scalar=ssq[b][:, 0:1], in1=gB[:],
                                                                              op0=ALU.mult, op1=ALU.mult),
                                  reads=[t_x[b], t_ssq[b], t_g], writes=[t_hb[b]])
                            for k in range(2):
                                for j in range(8):
                                    dk = k * 8 + j
                                    sy.op("pe", lambda e, b=b, k=k, j=j, dk=dk: e.transpose(pb[k][:, j * 128:(j + 1) * 128], hb[b][:, dk * 128:(dk + 1) * 128], identb[:]),
                                          reads=[t_hb[b], t_c], writes=[t_pb[k]])
                                evac(hT[:, k * 8:(k + 1) * 8, tt * 128:(tt + 1) * 128], pb[k][:].rearrange("p (a t) -> p a t", t=128),
                                     [t_pb[k]], [t_hT[tt]])
                        sy.barrier()

                    wst = ExitStack()
                    ab.enter_context(wst)
                    wring = [sb(wst, "wring%d" % i, [128, 16, 256], BF16) for i in range(4)]
                    t_wr = toks(4)
                    wr = {"n": 0}

                    def wload(view, c0, ncols=256):
                        i = wr["n"] % 4
                        wr["n"] += 1
                        sy.dma(wring[i][:, :, 0:ncols], view[:, :, c0:c0 + ncols], writes=[t_wr[i]])
                        return wring[i], t_wr[i]

                    pfr = {"n": 0}

                    def next_pf(lo=0, hi=6):
                        i = lo + pfr["n"] % (hi - lo)
                        pfr["n"] += 1
                        return pf[i], t_pf[i]

                    sy.enabled = "B" in PH
                    with ExitStack() as st:
                        NB_ = 2
                        xpad = [sb(st, "xpad%d" % i, [128, 515]) for i in range(NB_)]
                        xc = [sb(st, "xc%d" % i, [128, 512]) for i in range(NB_)]
                        xcb = [sb(st, "xcb%d" % i, [128, 512], BF16) for i in range(NB_)]
                        rr_ = [sb(st, "rg_r%d" % i, [128, 512]) for i in range(NB_)]
                        ri_ = [sb(st, "rg_i%d" % i, [128, 512]) for i in range(NB_)]
                        ra_ = [sb(st, "rg_a%d" % i, [128, 512]) for i in range(NB_)]
                        ra2_ = [sb(st, "rg_a2%d" % i, [128, 512]) for i in range(NB_)]
                        hsq = [sb(st, "hsq%d" % i, [128, 512]) for i in range(NB_)]
                        sgr = [sb(st, "sgr%d" % i, [128, 256]) for i in range(NB_)]
                        t_xpad = toks(NB_); t_xc = toks(NB_); t_xcb = toks(NB_); t_r = toks(NB_); t_i = toks(NB_)
                        t_a = toks(NB_); t_a2 = toks(NB_); t_hsq = toks(NB_); t_sgr = toks(NB_)
                        iters = [(cp, cc, hf) for cp in range(8) for cc in range(2) for hf in range(2)]
                        wcache = {}
                        st_ = {}

                        def rnn_front(k):
                            cp, cc, hf = iters[k]
                            c = cp * 2 + cc
                            b = k % NB_
                            if cp not in wcache:
                                wcache[cp] = (wload(Win_v, C_XR + cp * 256), wload(Win_v, C_GR + cp * 256))
                            (wx, t_wx), (wg, t_wg) = wcache[cp]
                            p_x, t_px = next_pf()
                            for dk in range(16):
                                sy.op("pe", lambda e, dk=dk: e.matmul(p_x[:], lhsT=wx[:, dk, cc * 128:(cc + 1) * 128], rhs=hT[:, dk, hf * 512:(hf + 1) * 512],
                                                                      start=(dk == 0), stop=(dk == 15)),
                                      reads=[t_wx] + t_hT[hf * 4:(hf + 1) * 4], writes=[t_px])
                            p_g, t_pg = next_pf()
                            for dk in range(16):
                                sy.op("pe", lambda e, dk=dk: e.matmul(p_g[:, 0:256].rearrange("p (a t) -> p a t", t=128), lhsT=wg[:, dk, cc * 128:(cc + 1) * 128],
                                                                      rhs=hTo[:, dk, hf * 2:(hf + 1) * 2, 1, :], start=(dk == 0), stop=(dk == 15)),
                                      reads=[t_wg] + t_hT[hf * 4:(hf + 1) * 4], writes=[t_pg])
                            sy.op("dve", lambda e: e.tensor_copy(out=xpad[b][:, 0:3], in_=halo[:, c, :]), reads=[t_halo[c]], writes=[t_xpad[b]])
                            sy.op("act", lambda e: e.activation(out=xpad[b][:, 3:515], in_=p_x[:], func=AF.Copy), reads=[t_px], writes=[t_xpad[b]])
                            sy.op("dve", lambda e: e.tensor_copy(out=halo[:, c, :], in_=xpad[b][:, 512:515]), reads=[t_xpad[b]], writes=[t_halo[c]])
                            sy.op("dve", lambda e: e.tensor_scalar(out=xc[b][:], in0=xpad[b][:, 3:515], scalar1=rnnp[:, 3, c:c + 1], scalar2=rnnp[:, 4, c:c + 1],
                                                                   op0=ALU.mult, op1=ALU.add), reads=[t_xpad[b], t_c], writes=[t_xc[b]])
                            for kk in range(3):
                                sy.op("dve", lambda e, kk=kk: e.scalar_tensor_tensor(out=xc[b][:], in0=xpad[b][:, kk:kk + 512], scalar=rnnp[:, kk, c:c + 1], in1=xc[b][:],
                                                                                   op0=ALU.mult, op1=ALU.add), reads=[t_xpad[b], t_xc[b]], writes=[t_xc[b]])
                            sy.op("act", lambda e: e.activation(out=xcb[b][:], in_=xc[b][:], func=AF.Copy), reads=[t_xc[b]], writes=[t_xcb[b]])
                            sy.op("act", lambda e: e.activation(out=sgr[b][:], in_=p_g[:, 0:256], func=AF.Sigmoid), reads=[t_pg], writes=[t_sgr[b]])

                        def rnn_back(k):
                            cp, cc, hf = iters[k]
                            c = cp * 2 + cc
                            b = k % NB_
                            p_r, t_pr = next_pf()
                            sy.op("pe", lambda e: e.matmul(p_r[:], lhsT=rgwa[:, c, :], rhs=xcb[b][:], start=True, stop=True), reads=[t_xcb[b], t_c], writes=[t_pr])
                            p_i, t_pi = next_pf()
                            sy.op("pe", lambda e: e.matmul(p_i[:], lhsT=rgwx[:, c, :], rhs=xcb[b][:], start=True, stop=True), reads=[t_xcb[b], t_c], writes=[t_pi])
                            sy.op("act", lambda e: e.activation(out=rr_[b][:], in_=p_r[:], func=AF.Sigmoid, bias=rnnp[:, 5, c:c + 1], scale=1.0), reads=[t_pr, t_c], writes=[t_r[b]])
                            sy.op("act", lambda e: e.activation(out=ri_[b][:], in_=p_i[:], func=AF.Sigmoid, bias=rnnp[:, 6, c:c + 1], scale=1.0), reads=[t_pi, t_c], writes=[t_i[b]])
                            sy.op("act", lambda e: e.activation(out=ra_[b][:], in_=rr_[b][:], func=AF.Exp, scale=cA[:, c:c + 1]), reads=[t_r[b], t_c], writes=[t_a[b]])
                            sy.op("act", lambda e: e.activation(out=ra2_[b][:], in_=rr_[b][:], func=AF.Exp, scale=cA2[:, c:c + 1]), reads=[t_r[b], t_c], writes=[t_a2[b]])
                            sy.op("act", lambda e: e.activation(out=ra2_[b][:], in_=ra2_[b][:], func=AF.Sqrt, bias=oneT[:], scale=-1.0), reads=[t_a2[b], t_c], writes=[t_a2[b]])
                            sy.op("dve", lambda e: e.tensor_tensor(out=ri_[b][:], in0=ri_[b][:], in1=xc[b][:], op=ALU.mult), reads=[t_i[b], t_xc[b]], writes=[t_i[b]])
                            sy.op("dve", lambda e: e.tensor_tensor(out=ri_[b][:], in0=ri_[b][:], in1=ra2_[b][:], op=ALU.mult), reads=[t_i[b], t_a2[b]], writes=[t_i[b]])
                            if g == 0 and hf == 0:
                                sy.op("dve", lambda e: e.tensor_tensor(out=ri_[b][:, 0:128], in0=ri_[b][:, 0:128], in1=vrow[:], op=ALU.mult), reads=[t_i[b], t_c], writes=[t_i[b]])
                            sy.op("dve", lambda e: e.tensor_tensor_scan(out=hsq[b][:], data0=ra_[b][:], data1=ri_[b][:], initial=hstate[:, c:c + 1], op0=ALU.mult, op1=ALU.add),
                                  reads=[t_a[b], t_i[b], t_hst[c]], writes=[t_hsq[b]])
                            sy.op("dve", lambda e: e.tensor_copy(out=hstate[:, c:c + 1], in_=hsq[b][:, 511:512]), reads=[t_hsq[b]], writes=[t_hst[c]])
                            sy.op("dve", lambda e: e.tensor_tensor(out=mixb[:, c, hf * 256:(hf + 1) * 256].rearrange("p (a t) -> p a t", t=128),
                                                                   in0=sgr[b][:].rearrange("p (a t) -> p a t", t=128),
                                                                   in1=hsq[b][:].rearrange("p (a two t) -> p a two t", two=2, t=128)[:, :, 1, :], op=ALU.mult),
                                  reads=[t_sgr[b], t_hsq[b]], writes=[t_mixb[c]])

                        rnn_front(0)
                        for k in range(len(iters)):
                            if k + 1 < len(iters):
                                rnn_front(k + 1)
                            rnn_back(k)
                        sy.barrier()

                    sy.enabled = "b" in PH or "B" in PH
                    with ExitStack() as st:
                        ckvT = sb(st, "ckvT", [128, 4, 1024], BF16)
                        t_ckvT = toks(8)
                        ckvn = [sb(st, "ckvn%d" % i, [128, 512], BF16) for i in range(2)]
                        sq2 = [sb(st, "sq2_%d" % i, [128, 1]) for i in range(2)]
                        junk = sb(st, "junkB", [128, 512], BF16)
                        kcen = [sb(st, "kcen%d" % i, [128, 64]) for i in range(2)]
                        kin2 = [sb(st, "kin2_%d" % i, [128, 128], BF16) for i in range(2)]
                        ksm = [sb(st, "ksm%d" % i, [128, 4]) for i in range(2)]
                        wI = sb(st, "wI", [128, 4, 16])
                        wuk = [sb(st, "wuk%d" % i, [128, 4, 128], BF16) for i in range(2)]
                        wuv = sb(st, "wuv", [128, 4, D], BF16)
                        ktw = [sb(st, "ktw%d" % i, [128, 1024], BF16) for i in range(2)]
                        vw = [sb(st, "vw%d" % i, [128, D], BF16) for i in range(2)]
                        t_ckvn = toks(2); t_sq2 = toks(2); t_junk = Tok(); t_kcen = toks(2); t_kin2 = toks(2); t_ksm = toks(2)
                        t_wuk = toks(2); t_wuv = Tok(); t_ktw = toks(2); t_vw = toks(2)
                        sy.dma(wuv[:], Wuv_v[:, :, :], writes=[t_wuv])

                        def fm_proj(c_base, nchunks, dst, t_dst, sig):
                            for cp in range(nchunks // 2):
                                wb, t_wb = wload(Win_v, c_base + cp * 256)
                                for cc in range(2):
                                    c = cp * 2 + cc
                                    p_, t_p = next_pf()
                                    for dk in range(16):
                                        sy.op("pe", lambda e, dk=dk, p_=p_, wb=wb, cc=cc: e.matmul(p_[:].rearrange("p (a t) -> p a t", t=128), lhsT=wb[:, dk, cc * 128:(cc + 1) * 128],
                                                                                      rhs=hTo[:, dk, :, 1, :], start=(dk == 0), stop=(dk == 15)),
                                              reads=[t_wb] + t_hT, writes=[t_p])
                                    if sig:
                                        sy.op("act", lambda e, p_=p_, c=c: e.activation(out=dst[:, c, :], in_=p_[:], func=AF.Sigmoid), reads=[t_p], writes=[t_dst[c]])
                                    else:
                                        evac(dst[:, c, :], p_[:], [t_p], [t_dst[c]])
                        fm_proj(C_Q, 16, qT, t_qT, False)
                        fm_proj(C_QI, 8, qiT, t_qiT, False)
                        fm_proj(C_GA, 16, sga, t_sga, True)

                        wc0, t_wc0 = wload(Win_v, C_CKV)
                        wc1, t_wc1 = wload(Win_v, C_CKV + 256)
                        for tt in range(8):
                            b = tt % 2
                            p_, t_p = next_pf()
                            for pc, (wc, t_wc) in enumerate([(wc0, t_wc0), (wc1, t_wc1)]):
                                for dk in range(16):
                                    sy.op("pe", lambda e, dk=dk, p_=p_, wc=wc, pc=pc, tt=tt: e.matmul(p_[:, pc * 256:(pc + 1) * 256], lhsT=hT[:, dk, tt * 128:(tt + 1) * 128], rhs=wc[:, dk, :],
                                                                                        start=(dk == 0), stop=(dk == 15)),
                                          reads=[t_wc, t_hT[tt]], writes=[t_p])
                            sy.op("act", lambda e, p_=p_, b=b: e.activation(out=junk[:], in_=p_[:], func=AF.Square, accum_out=sq2[b][:]), reads=[t_p], writes=[t_junk, t_sq2[b]])
                            rms_rstd(sq2[b][:], t_sq2[b], 512)
                            sy.op("dve", lambda e, p_=p_, b=b: e.scalar_tensor_tensor(out=ckvn[b][:], in0=p_[:], scalar=sq2[b][:, 0:1], in1=gkvB[:], op0=ALU.mult, op1=ALU.mult),
                                  reads=[t_p, t_sq2[b], t_c], writes=[t_ckvn[b]])
                            k = tt % 2
                            for cc in range(4):
                                sy.op("pe", lambda e, b=b, k=k, cc=cc: e.transpose(pb[k][:, cc * 128:(cc + 1) * 128], ckvn[b][:, cc * 128:(cc + 1) * 128], identb[:]),
                                      reads=[t_ckvn[b], t_c], writes=[t_pb[k]])
                            evac(ckvT[:, :, tt * 128:(tt + 1) * 128], pb[k][:, 0:512].rearrange("p (a t) -> p a t", t=128), [t_pb[k]], [t_ckvT[tt]])

                        wk, t_wk = wload(Win_v, C_KI, 80)
                        for tt in range(8):
                            b = tt % 2
                            p_, t_p = next_pf()
                            for dk in range(16):
                                sy.op("pe", lambda e, dk=dk, p_=p_, tt=tt: e.matmul(p_[:, 0:80], lhsT=hT[:, dk, tt * 128:(tt + 1) * 128], rhs=wk[:, dk, 0:80], start=(dk == 0), stop=(dk == 15)),
                                      reads=[t_wk, t_hT[tt]], writes=[t_p])
                            sy.op("dve", lambda e, p_=p_, b=b: e.tensor_reduce(out=ksm[b][:, 0:1], in_=p_[:, 0:64], axis=AX.X, op=ALU.add), reads=[t_p], writes=[t_ksm[b]])
                            sy.op("dve", lambda e, b=b: e.tensor_scalar(out=ksm[b][:, 1:2], in0=ksm[b][:, 0:1], scalar1=-1.0 / 64.0, scalar2=None, op0=ALU.mult), reads=[t_ksm[b]], writes=[t_ksm[b]])
                            sy.op("dve", lambda e, p_=p_, b=b: e.tensor_scalar(out=kcen[b][:], in0=p_[:, 0:64], scalar1=ksm[b][:, 1:2], scalar2=None, op0=ALU.add), reads=[t_p, t_ksm[b]], writes=[t_kcen[b]])
                            sy.op("act", lambda e, b=b: e.activation(out=junk[:, 0:64], in_=kcen[b][:], func=AF.Square, accum_out=ksm[b][:, 2:3]), reads=[t_kcen[b]], writes=[t_junk, t_ksm[b]])
                            rms_rstd(ksm[b][:, 2:3], t_ksm[b], 64)
                            sy.op("dve", lambda e, b=b: e.scalar_tensor_tensor(out=kcen[b][:], in0=kcen[b][:], scalar=ksm[b][:, 2:3], in1=idxgB[:], op0=ALU.mult, op1=ALU.mult),
                                  reads=[t_kcen[b], t_ksm[b], t_c], writes=[t_kcen[b]])
                            sy.op("dve", lambda e, b=b: e.tensor_tensor(out=kin2[b][:, 0:64], in0=kcen[b][:], in1=idxbB[:], op=ALU.add), reads=[t_kcen[b], t_c], writes=[t_kin2[b]])
                            sy.op("dve", lambda e, b=b: e.tensor_tensor(out=kin2[b][:, 64:128], in0=kcen[b][:], in1=idxbB[:], op=ALU.add), reads=[t_kcen[b], t_c], writes=[t_kin2[b]])
                            k = tt % 2
                            sy.op("pe", lambda e, b=b, k=k: e.transpose(pb[k][:, 0:128], kin2[b][:], identb[:]), reads=[t_kin2[b], t_c], writes=[t_pb[k]])
                            blk = g * 8 + tt
                            evac(kiT2[:, blk * 128:(blk + 1) * 128], pb[k][:, 0:128], [t_pb[k]], [t_kiT[blk]])
                            if tt % 2 == 1:
                                jj = tt // 2
                                sy.op("dve", lambda e, p_=p_, jj=jj: e.tensor_scalar(out=wI[:, jj, :], in0=p_[:, 64:80], scalar1=0.25 * 0.125, scalar2=None, op0=ALU.mult), reads=[t_p], writes=[t_w[jj]])
                                sy.op("act", lambda e, jj=jj: e.activation(out=absw[:, jj, :], in_=wI[:, jj, :], func=AF.Abs), reads=[t_w[jj]], writes=[t_w[jj]])
                                sy.op("act", lambda e, jj=jj: e.activation(out=sgnw[:, jj, :], in_=wI[:, jj, :], func=AF.Sign), reads=[t_w[jj]], writes=[t_w[jj]])

                        for h in range(16):
                            b = h % 2
                            sy.dma(wuk[b][:].rearrange("p a b -> p (a b)"), Wuk_bf[h, :, :], writes=[t_wuk[b]])
                            for nt in range(2):
                                p_, t_p = next_pf()
                                for cc in range(4):
                                    sy.op("pe", lambda e, p_=p_, b=b, cc=cc, nt=nt: e.matmul(p_[:], lhsT=wuk[b][:, cc, :], rhs=ckvT[:, cc, nt * 512:(nt + 1) * 512], start=(cc == 0), stop=(cc == 3)),
                                          reads=[t_wuk[b]] + t_ckvT[nt * 4:(nt + 1) * 4], writes=[t_p])
                                evac(ktw[b][:, nt * 512:(nt + 1) * 512], p_[:], [t_p], [t_ktw[b]])
                            sy.dma(KT_s[h, :, g * 1024:(g + 1) * 1024], ktw[b][:], reads=[t_ktw[b]], writes=[t_KT[g][h]])
                        for stl in range(8):
                            b = stl % 2
                            for nb in range(4):
                                p_, t_p = next_pf()
                                for cc in range(4):
                                    sy.op("pe", lambda e, p_=p_, cc=cc, nb=nb, stl=stl: e.matmul(p_[:], lhsT=ckvT[:, cc, stl * 128:(stl + 1) * 128], rhs=wuv[:, cc, nb * 512:(nb + 1) * 512], start=(cc == 0), stop=(cc == 3)),
                                          reads=[t_wuv, t_ckvT[stl]], writes=[t_p])
                                evac(vw[b][:, nb * 512:(nb + 1) * 512], p_[:], [t_p], [t_vw[b]])
                            r0 = g * 1024 + stl * 128
                            sy.dma(VV_s[r0:r0 + 128, :], vw[b][:], reads=[t_vw[b]], writes=[t_VV[g][stl]])
                        sy.barrier()
                    sy.barrier()

                sy.enabled = "C" in PH
                NST = 8 * g + 8
                with ExitStack() as cd:
                    maskbT = sb(cd, "maskbT", [128, NST, 512], BF16)
                    t_mb = [[Tok() for _ in range(4)] for _ in range(NST // 8)]
                    pf = [psum(cd, "pfC%d" % i, [128, 512]) for i in range(6)]
                    pb = [psum(cd, "pbC%d" % i, [128, 1024], BF16) for i in range(2)]
                    t_pf = toks(6)
                    t_pb = toks(2)
                    with ExitStack() as st:
                        isc = sb(st, "isc", [128, NST * 128])
                        junk8 = sb(st, "junk8", [128, NST * 128], FP8)
                        maskq = [sb(st, "maskq%d" % i, [128, 1024], BF16) for i in range(2)]
                        rtmp = [sb(st, "rtmp%d" % i, [128, 1024]) for i in range(2)]
                        bis = sb(st, "bis", [128, 8])
                        half = sb(st, "half", [128, NBIS + 1])
                        bis2 = sb(st, "bis2", [128, 2])
                        t_mid = Tok(); t_cnt = Tok(); t_cnt2 = Tok(); t_comb = Tok(); t_junk8b = Tok()
                        t_isc = Tok(); t_junk8 = Tok(); t_mq = toks(2); t_rt = toks(2); t_bis = Tok(); t_half = Tok()
                        rti = 0
                        pfi = 0
                        for jj in range(4):
                            pblk = 8 * g + 2 * jj + 1
                            nk = (pblk + 1) * 128
                            nts = [(nt, min(512, nk - nt * 512)) for nt in range((nk + 511) // 512)]
                            for pr in range(0, len(nts), 2):
                                grp = nts[pr:pr + 2]
                                c0 = grp[0][0] * 512
                                wt = sum(w_ for _, w_ in grp)
                                for h in range(16):
                                    hp = (h % 2) * 64
                                    rb = rti % 2
                                    rti += 1
                                    off = 0
                                    for (nt, w) in grp:
                                        p_, t_p = pf[pfi % 6], t_pf[pfi % 6]
                                        pfi += 1
                                        kblks = t_kiT[nt * 4:nt * 4 + w // 128]
                                        sy.op("pe", lambda e, p_=p_, nt=nt, w=w: e.matmul(p_[:, 0:w], lhsT=qiT[hp:hp + 64, h // 2, jj * 128:(jj + 1) * 128],
                                                                               rhs=kiT2[hp:hp + 64, nt * 512:nt * 512 + w], start=True, stop=True),
                                              reads=[t_qiT[h // 2]] + kblks, writes=[t_p])
                                        sy.op("act", lambda e, p_=p_, off=off, w=w: e.activation(out=rtmp[rb][:, off:off + w], in_=p_[:, 0:w], func=AF.Relu, scale=absw[:, jj, h:h + 1]),
                                              reads=[t_p, t_w[jj]], writes=[t_rt[rb]])
                                        off += w
                                    if h == 0:
                                        sy.op("dve", lambda e: e.tensor_scalar(out=isc[:, c0:c0 + wt], in0=rtmp[rb][:, 0:wt], scalar1=sgnw[:, jj, 0:1], scalar2=None, op0=ALU.mult),
                                              reads=[t_rt[rb], t_w[jj]], writes=[t_isc])
                                    else:
                                        sy.op("dve", lambda e: e.scalar_tensor_tensor(out=isc[:, c0:c0 + wt], in0=rtmp[rb][:, 0:wt], scalar=sgnw[:, jj, h:h + 1],
                                                                                     in1=isc[:, c0:c0 + wt], op0=ALU.mult, op1=ALU.add),
                                              reads=[t_rt[rb], t_w[jj], t_isc], writes=[t_isc])
                            sy.op("dve", lambda e, nk=nk: e.tensor_reduce(out=bis[:, 0:1], in_=isc[:, 0:nk], axis=AX.X, op=ALU.max, apply_absolute_value=True), reads=[t_isc], writes=[t_bis])
                            sy.op("dve", lambda e: e.tensor_scalar(out=bis[:, 1:2], in0=bis[:, 0:1], scalar1=2.0, scalar2=2.0, op0=ALU.mult, op1=ALU.add), reads=[t_bis], writes=[t_bis])
                            sy.op("dve", lambda e: e.tensor_scalar(out=half[:], in0=pow2[:], scalar1=bis[:, 1:2], scalar2=None, op0=ALU.mult), reads=[t_bis, t_c], writes=[t_half])
                            sy.op("dve", lambda e: e.tensor_scalar(out=bis[:, 2:3], in0=bis[:, 1:2], scalar1=-0.5, scalar2=half[:, 0:1], op0=ALU.mult, op1=ALU.add), reads=[t_bis, t_half], writes=[t_mid])
                            sy.op("dve", lambda e, pblk=pblk: e.tensor_tensor(out=isc[:, pblk * 128:(pblk + 1) * 128], in0=isc[:, pblk * 128:(pblk + 1) * 128], in1=tri[:], op=ALU.add), reads=[t_isc, t_c], writes=[t_isc])
                            if g == 0:
                                sy.op("dve", lambda e: e.tensor_tensor(out=isc[:, 0:128], in0=isc[:, 0:128], in1=kb0[:], op=ALU.add), reads=[t_isc, t_c], writes=[t_isc])
                            n1 = ((nk // 2 + 127) // 128) * 128
                            n2 = nk - n1
                            for k in range(NBIS):
                                sy.op("dve", lambda e: e.tensor_scalar(out=junk8[:, 0:n1], in0=isc[:, 0:n1], scalar1=bis[:, 2:3], scalar2=None, op0=ALU.is_ge, op1=ALU.add, accum_out=bis[:, 3:4], saturate=False),
                                      reads=[t_isc, t_mid], writes=[t_junk8, t_cnt])
                                sy.op("act", lambda e: e.activation(out=junk8[:, n1:nk], in_=isc[:, n1:nk], func=AF.Sign, bias=bis[:, 2:3], scale=-1.0, accum_out=bis2[:, 0:1], saturate=False),
                                      reads=[t_isc, t_mid], writes=[t_junk8b, t_cnt2])
                                kk = k + 1 if k < NBIS - 1 else k
                                sy.op("dve", lambda e, kk=kk: e.tensor_tensor(out=bis[:, 4:5], in0=bis[:, 2:3], in1=half[:, kk:kk + 1], op=ALU.subtract), reads=[t_mid, t_half], writes=[t_bis])
                                sy.op("dve", lambda e: e.scalar_tensor_tensor(out=bis[:, 6:7], in0=bis[:, 3:4], scalar=2.0, in1=bis2[:, 0:1], op0=ALU.mult, op1=ALU.subtract),
                                      reads=[t_cnt, t_cnt2], writes=[t_comb])
                                sy.op("dve", lambda e, k=k: e.scalar_tensor_tensor(out=bis[:, 5:6], in0=bis[:, 6:7], scalar=2.0 * TOPK - n2, in1=half[:, k:k + 1], op0=ALU.is_ge, op1=ALU.mult),
                                      reads=[t_comb, t_half], writes=[t_bis])
                                sy.op("dve", lambda e: e.tensor_tensor(out=bis[:, 2:3], in0=bis[:, 4:5], in1=bis[:, 5:6], op=ALU.add), reads=[t_bis], writes=[t_mid])
                            for kc in range((nk + 1023) // 1024):
                                w = min(1024, nk - kc * 1024)
                                mq = kc % 2
                                sy.op("dve", lambda e, mq=mq, kc=kc, w=w: e.tensor_scalar(out=maskq[mq][:, 0:w], in0=isc[:, kc * 1024:kc * 1024 + w], scalar1=bis[:, 2:3], scalar2=None, op0=ALU.is_ge),
                                      reads=[t_isc, t_mid], writes=[t_mq[mq]])
                                k = kc % 2
                                for j in range(w // 128):
                                    sy.op("pe", lambda e, k=k, j=j, mq=mq: e.transpose(pb[k][:, j * 128:(j + 1) * 128], maskq[mq][:, j * 128:(j + 1) * 128], identb[:]),
                                          reads=[t_mq[mq], t_c], writes=[t_pb[k]])
                                sy.op("pool" if False else "dve", lambda e, k=k, kc=kc, w=w, jj=jj: e.tensor_scalar(out=maskbT[:, kc * 8:kc * 8 + w // 128, jj * 128:(jj + 1) * 128],
                                                                                          in0=pb[k][:, 0:w].rearrange("p (a t) -> p a t", t=128), scalar1=1.0, scalar2=None, op0=ALU.mult),
                                      reads=[t_pb[k]], writes=[t_mb[kc][jj]])
                            if pblk + 1 < NST:
                                c0 = (pblk + 1) // 8
                                sy.op("pool", lambda e, pblk=pblk, jj=jj: e.memset(maskbT[:, pblk + 1:NST, jj * 128:(jj + 1) * 128], 0.0),
                                      reads=[t_mb[c][jj] for c in range(c0, NST // 8)], writes=[t_mb[c][jj] for c in range(c0, NST // 8)])
                        sy.barrier()

                    with ExitStack() as st:
                        sy.enabled = "D" in PH
                        KTc = [sb(st, "KTc%d" % i, [128, 1024], BF16) for i in range(4)]
                        Vc = [sb(st, "Vc%d" % i, [128, 8, 128], BF16) for i in range(4)]
                        pT = [sb(st, "pT%d" % i, [128, 512], BF16) for i in range(6)]
                        rden = [sb(st, "rden%d" % i, [128, 512]) for i in range(2)]
                        ty = [sb(st, "ty%d" % i, [128, 512]) for i in range(2)]
                        t_KTc = toks(4); t_Vc = toks(4); t_pT = toks(6); t_rden = toks(2); t_ty = toks(2)
                        VV_v = VV_s.rearrange("(a p) n -> p a n", p=128)
                        li = 0
                        pti = 0
                        scale = 128.0 ** -0.5
                        NKC = NST // 8
                        tiles = [(h, kc, s_) for h in range(16) for kc in range(NKC) for s_ in range(8)]
                        chunk_buf = {}

                        def load_chunk(h, kc):
                            nonlocal_li = len(chunk_buf)
                            b = nonlocal_li % 4
                            chunk_buf[(h, kc)] = b
                            sy.dma(KTc[b][:], KT_s[h, :, kc * 1024:(kc + 1) * 1024], reads=[t_KT[kc][h]], writes=[t_KTc[b]])
                            sy.dma(Vc[b][:], VV_v[:, kc * 8:(kc + 1) * 8, h * 128:(h + 1) * 128], reads=t_VV[kc], writes=[t_Vc[b]])

                        def emit_logits(idx):
                            h, kc, s_ = tiles[idx]
                            if (h, kc) not in chunk_buf:
                                load_chunk(h, kc)
                                nxt = idx + 8 - (idx % 8)
                                if nxt < len(tiles) and (tiles[nxt][0], tiles[nxt][1]) not in chunk_buf:
                                    load_chunk(tiles[nxt][0], tiles[nxt][1])
                            b = chunk_buf[(h, kc)]
                            stg_ = kc * 8 + s_
                            pL, t_pL = pf[idx % 4], t_pf[idx % 4]
                            sy.op("pe", lambda e: e.matmul(pL[:], lhsT=KTc[b][:, s_ * 128:(s_ + 1) * 128], rhs=qT[:, h, :], start=True, stop=True),
                                  reads=[t_KTc[b], t_qT[h]], writes=[t_pL])

                        LA = 3
                        for i0 in range(min(LA, len(tiles))):
                            emit_logits(i0)
                        for idx, (h, kc, s_) in enumerate(tiles):
                            if idx + LA < len(tiles):
                                emit_logits(idx + LA)
                            b = chunk_buf[(h, kc)]
                            stg_ = kc * 8 + s_
                            pL, t_pL = pf[idx % 4], t_pf[idx % 4]
                            pO, t_pO = pf[4], t_pf[4]
                            pD, t_pD = pf[5], t_pf[5]
                            pi = idx % 6
                            sy.op("act", lambda e: e.activation(out=pT[pi][:], in_=pL[:], func=AF.Exp, scale=scale), reads=[t_pL], writes=[t_pT[pi]])
                            sy.op("dve", lambda e: e.tensor_tensor(out=pT[pi][:], in0=pT[pi][:], in1=maskbT[:, stg_, :], op=ALU.mult), reads=[t_pT[pi]] + t_mb[kc], writes=[t_pT[pi]])
                            sy.op("pe", lambda e: e.matmul(pO[:], lhsT=Vc[b][:, s_, :], rhs=pT[pi][:], start=(stg_ == 0), stop=(stg_ == NST - 1)),
                                  reads=[t_Vc[b], t_pT[pi]], writes=[t_pO])
                            sy.op("pe", lambda e: e.matmul(pD[:], lhsT=onesb[:], rhs=pT[pi][:], start=(stg_ == 0), stop=(stg_ == NST - 1)),
                                  reads=[t_c, t_pT[pi]], writes=[t_pD])
                            if stg_ == NST - 1:
                                b2 = h % 2
                                sy.op("dve", lambda e: e.reciprocal(out=rden[b2][:], in_=pD[:]), reads=[t_pD], writes=[t_rden[b2]])
                                sy.op("dve", lambda e: e.tensor_tensor(out=ty[b2][:], in0=pO[:], in1=rden[b2][:], op=ALU.mult), reads=[t_pO, t_rden[b2]], writes=[t_ty[b2]])
                                sy.op("pool", lambda e: e.tensor_tensor(out=ty[b2][:], in0=ty[b2][:], in1=sga[:, h, :], op=ALU.mult), reads=[t_ty[b2], t_sga[h]], writes=[t_ty[b2]])
                                sy.op("pool", lambda e: e.tensor_tensor(out=mixb[:, h, :], in0=ty[b2][:], in1=mixb[:, h, :], op=ALU.add), reads=[t_ty[b2], t_mixb[h]], writes=[t_mixb[h]])
                        sy.barrier()
                    sy.barrier()

                sy.enabled = "E" in PH
                ef = ExitStack()
                acc = sb(ef, "acc", [128, 4, D], side="right")
                xnT = sb(ef, "xnT", [128, 16, 512], BF16, side="right")
                t_acc = [[Tok() for _ in range(8)] for _ in range(4)]
                t_xnT = toks(4)
                with ExitStack() as st:
                    pf = [psum(st, "pfE%d" % i, [128, 512]) for i in range(6)]
                    t_pf = toks(6)
                    wring = [sb(st, "wringE%d" % i, [128, 16, 256], BF16) for i in range(4)]
                    t_wr = toks(4)
                    for ts in range(4):
                        r0 = (8 * g + 2 * ts + 1) * 128
                        sy.dma(acc[:, ts, :], xs[r0:r0 + 128, :], writes=t_acc[ts])
                    pfi = 0
                    for nb in range(8):
                        i = nb % 4
                        sy.dma(wring[i][:], Wo_v[:, :, nb * 256:(nb + 1) * 256], writes=[t_wr[i]])
                        for ts in range(4):
                            p_, t_p = pf[pfi % 6], t_pf[pfi % 6]
                            pfi += 1
                            for cc in range(16):
                                sy.op("pe", lambda e, p_=p_, cc=cc, ts=ts, i=i: e.matmul(p_[:, 0:256], lhsT=mixb[:, cc, ts * 128:(ts + 1) * 128], rhs=wring[i][:, cc, :], start=(cc == 0), stop=(cc == 15)),
                                      reads=[t_mixb[cc], t_wr[i]], writes=[t_p])
                            sy.op("dve", lambda e, p_=p_, ts=ts, nb=nb: e.tensor_tensor(out=acc[:, ts, nb * 256:(nb + 1) * 256], in0=p_[:, 0:256], in1=acc[:, ts, nb * 256:(nb + 1) * 256], op=ALU.add),
                                  reads=[t_p, t_acc[ts][nb]], writes=[t_acc[ts][nb]])
                    sy.barrier()
            sy.barrier()
            with ef:
                with ExitStack() as st:
                    pb = [psum(st, "pbE%d" % i, [128, 1024], BF16) for i in range(2)]
                    t_pb = toks(2)
                    gB = sb(st, "gBf", [128, D])
                    xnb = [sb(st, "xnb%d" % i, [128, D], BF16) for i in range(2)]
                    ssq = [sb(st, "ssqE%d" % i, [128, 1]) for i in range(2)]
                    t_g = Tok(); t_xnb = toks(2); t_ssq = toks(2)
                    sy.dma(gB[:], g_ffn_d.partition_broadcast(128), writes=[t_g])
                    for ts in range(4):
                        b = ts % 2
                        sy.op("act", lambda e, b=b, ts=ts: e.activation(out=xnb[b][:], in_=acc[:, ts, :], func=AF.Square, accum_out=ssq[b][:]), reads=t_acc[ts], writes=[t_xnb[b], t_ssq[b]])
                        rms_rstd(ssq[b][:], t_ssq[b], D)
                        sy.op("dve", lambda e, b=b, ts=ts: e.scalar_tensor_tensor(out=xnb[b][:], in0=acc[:, ts, :], scalar=ssq[b][:, 0:1], in1=gB[:], op0=ALU.mult, op1=ALU.mult),
                              reads=t_acc[ts] + [t_ssq[b], t_g], writes=[t_xnb[b]])
                        for k in range(2):
                            for j in range(8):
                                dk = k * 8 + j
                                sy.op("pe", lambda e, b=b, k=k, j=j, dk=dk: e.transpose(pb[k][:, j * 128:(j + 1) * 128], xnb[b][:, dk * 128:(dk + 1) * 128], identb[:]),
                                      reads=[t_xnb[b], t_c], writes=[t_pb[k]])
                            evac(xnT[:, k * 8:(k + 1) * 8, ts * 128:(ts + 1) * 128], pb[k][:].rearrange("p (a t) -> p a t", t=128), [t_pb[k]], [t_xnT[ts]])
                    sy.barrier()

                sy.enabled = "F" in PH or "f" in PH
                with ExitStack() as fs:
                    s1 = sb(fs, "s1", [128, 4, 8, 128])
                    s2 = sb(fs, "s2", [128, 4, 8, 128])
                    v1 = sb(fs, "v1", [128, 4, 8, 16])
                    v2 = sb(fs, "v2", [128, 4, 8, 16])
                    thra = sb(fs, "thra", [128, 4, 8])
                    E2t = sb(fs, "E2t", [128, 8, 128], BF16)
                    t_E2t = Tok()
                    rZ = sb(fs, "rZ", [128, 4, 8])
                    t_tab = toks(4)
                    pf = [psum(fs, "pfF%d" % i, [128, 512]) for i in range(8)]
                    t_pf = toks(8)
                    with ExitStack() as st:
                        qpT = sb(st, "qpT", [128, 16, 512], BF16)
                        t_qp = toks(16)
                        wring = [sb(st, "wringF%d" % i, [128, 16, 256], BF16) for i in range(4)]
                        t_wr = toks(4)
                        xw = sb(st, "xw", [128, 256])
                        cand = sb(st, "cand", [128, 8, 16, 16])
                        tops = sb(st, "tops", [128, 8, 16])
                        tmpz = sb(st, "tmpz", [128, 8, 16])
                        sm = sb(st, "sm", [128, 8, 4])
                        t_xw = Tok(); t_cand = Tok(); t_tops = Tok(); t_tmpz = Tok(); t_sm = Tok()
                        tmpE = sb(st, "tmpE", [128, 8, 128]); t_tmpE = Tok()
                        pfi = 0
                        for cp in range(8):
                            i = cp % 4
                            sy.dma(wring[i][:], Wq_v[:, :, cp * 256:(cp + 1) * 256], writes=[t_wr[i]])
                            for cc in range(2):
                                c = cp * 2 + cc
                                p_, t_p = pf[pfi % 4], t_pf[pfi % 4]
                                pfi += 1
                                for dk in range(16):
                                    sy.op("pe", lambda e, p_=p_, dk=dk, i=i, cc=cc: e.matmul(p_[:], lhsT=wring[i][:, dk, cc * 128:(cc + 1) * 128], rhs=xnT[:, dk, :], start=(dk == 0), stop=(dk == 15)),
                                          reads=[t_wr[i]] + t_xnT, writes=[t_p])
                                evac(qpT[:, c, :], p_[:], [t_p], [t_qp[c]])
                        for ts in range(4):
                            for bk in range(4):
                                p_, t_p = pf[4 + bk], t_pf[4 + bk]
                                for j in range(4):
                                    c = bk * 4 + j
                                    sy.op("pe", lambda e, p_=p_, j=j, c=c, ts=ts: e.matmul(p_[:, j * 128:(j + 1) * 128], lhsT=qpT[:, c, ts * 128:(ts + 1) * 128], rhs=(k1T if c % 2 == 0 else k2T)[:],
                                                                               start=True, stop=True), reads=[t_qp[c], t_c], writes=[t_p])
                                pv_ = p_[:].rearrange("p (h f k) -> p h f k", h=2, f=2)
                                sy.op("act", lambda e, pv_=pv_, ts=ts, bk=bk: e.activation(out=s1[:, ts, 2 * bk:2 * bk + 2, :], in_=pv_[:, :, 0, :], func=AF.Copy), reads=[t_p], writes=[t_tab[ts]])
                                sy.op("act", lambda e, pv_=pv_, ts=ts, bk=bk: e.activation(out=s2[:, ts, 2 * bk:2 * bk + 2, :], in_=pv_[:, :, 1, :], func=AF.Copy), reads=[t_p], writes=[t_tab[ts]])
                            for (sX, vX) in ((s1, v1), (s2, v2)):
                                for h in range(8):
                                    sy.op("dve", lambda e, sX=sX, vX=vX, ts=ts, h=h: e.max(out=vX[:, ts, h, 0:8], in_=sX[:, ts, h, :]), reads=[t_tab[ts]], writes=[t_tab[ts]])
                                    sy.op("dve", lambda e, sX=sX, vX=vX, ts=ts, h=h: e.match_replace(out=xw[:, 0:128], in_to_replace=vX[:, ts, h, 0:8], in_values=sX[:, ts, h, :], imm_value=NEG),
                                          reads=[t_tab[ts]], writes=[t_xw])
                                    sy.op("dve", lambda e, vX=vX, ts=ts, h=h: e.max(out=vX[:, ts, h, 8:16], in_=xw[:, 0:128]), reads=[t_xw], writes=[t_tab[ts]])
                            sy.op("dve", lambda e, ts=ts: e.tensor_tensor(out=cand[:], in0=v1[:, ts, :, :].unsqueeze(3).to_broadcast([128, 8, 16, 16]),
                                                                    in1=v2[:, ts, :, :].unsqueeze(2).to_broadcast([128, 8, 16, 16]), op=ALU.add), reads=[t_tab[ts]], writes=[t_cand])
                            for h in range(8):
                                ch = cand[:, h, :, :].rearrange("p a b -> p (a b)")
                                sy.op("dve", lambda e, ch=ch, h=h: e.max(out=tops[:, h, 0:8], in_=ch), reads=[t_cand], writes=[t_tops])
                                sy.op("dve", lambda e, ch=ch, h=h: e.match_replace(out=xw[:], in_to_replace=tops[:, h, 0:8], in_values=ch, imm_value=NEG), reads=[t_cand, t_tops], writes=[t_xw])
                                sy.op("dve", lambda e, h=h: e.max(out=tops[:, h, 8:16], in_=xw[:]), reads=[t_xw], writes=[t_tops])
                            sy.op("dve", lambda e: e.tensor_tensor(out=tmpz[:], in0=tops[:], in1=tops[:, :, 0:1].to_broadcast([128, 8, 16]), op=ALU.subtract), reads=[t_tops], writes=[t_tmpz])
                            sy.op("act", lambda e: e.activation(out=tmpz[:], in_=tmpz[:], func=AF.Exp), reads=[t_tmpz], writes=[t_tmpz])
                            sy.op("dve", lambda e: e.tensor_reduce(out=sm[:, :, 0], in_=tmpz[:], axis=AX.X, op=ALU.add), reads=[t_tmpz], writes=[t_sm])
                            sy.op("act", lambda e: e.activation(out=sm[:, :, 2], in_=sm[:, :, 0], func=AF.Ln), reads=[t_sm], writes=[t_sm])
                            sy.op("dve", lambda e, ts=ts: e.tensor_tensor(out=rZ[:, ts, :], in0=sm[:, :, 2], in1=tops[:, :, 0], op=ALU.add), reads=[t_sm, t_tops], writes=[t_tab[ts]])
                            sy.op("act", lambda e: e.activation(out=sm[:, :, 1], in_=tops[:, :, 0], func=AF.Abs, scale=PEER_MARGIN), reads=[t_tops, t_sm], writes=[t_sm])
                            sy.op("dve", lambda e, ts=ts: e.tensor_tensor(out=thra[:, ts, :], in0=tops[:, :, 15], in1=sm[:, :, 1], op=ALU.subtract), reads=[t_tops, t_sm], writes=[t_tab[ts]])
                            if ts == 3:
                                sy.op("dve", lambda e: e.tensor_tensor(out=tmpE[:], in0=s2[:, 3, :, :], in1=v2[:, 3, :, 0:1].to_broadcast([128, 8, 128]), op=ALU.subtract), reads=[t_tab[3]], writes=[t_tmpE])
                                sy.op("act", lambda e: e.activation(out=E2t[:], in_=tmpE[:], func=AF.Exp), reads=[t_tmpE], writes=[t_E2t])
                        sy.barrier()

                    sy.enabled = "F" in PH
                    with ExitStack() as st:
                        uTb = [sb(st, "uTb%d" % i, [128, 16, 128], BF16) for i in range(4)]
                        vb = [sb(st, "vb%d" % i, [128, SG, 512], BF16) for i in range(2)]
                        actG = [sb(st, "actG%d" % i, [128, SG, 512], BF16) for i in range(2)]
                        gz = [sb(st, "gz%d" % i, [128, 512]) for i in range(2)]
                        Ab = [sb(st, "Ab%d" % i, [128, 16, 128], BF16) for i in range(4)]
                        Ew = [sb(st, "Ew%d" % i, [128, 32, 128], BF16) for i in range(2)]
                        t_Ew = [toks(32) for _ in range(2)]
                        thS = sb(st, "thS", [128, 2, 4, 8, SG])
                        w1S = sb(st, "w1S", [128, 2, 4, 8, SG])
                        w13S = sb(st, "w13S", [128, 2, 8, SG], BF16)
                        w13t = sb(st, "w13t", [128, 8, SG]); t_w13t = Tok()
                        t_uT = toks(4); t_vb = toks(2); t_gz = toks(2); t_Ab = toks(4)
                        t_actG = [[Tok() for _ in range(SG)] for _ in range(2)]
                        t_thS = toks(2)
                        NSG = 128 // SG

                        def sg_tables(sg_):
                            sb_ = sg_ % 2
                            i1s = slice(sg_ * SG, (sg_ + 1) * SG)
                            for ts in range(4):
                                sy.op("dve", lambda e, ts=ts: e.tensor_tensor(out=thS[:, sb_, ts, :, :], in0=thra[:, ts, :].unsqueeze(2).to_broadcast([128, 8, SG]), in1=s1[:, ts, :, i1s], op=ALU.subtract),
                                      reads=[t_tab[ts]], writes=[t_thS[sb_]])
                                sy.op("dve", lambda e, ts=ts: e.tensor_tensor(out=w1S[:, sb_, ts, :, :], in0=s1[:, ts, :, i1s], in1=rZ[:, ts, :].unsqueeze(2).to_broadcast([128, 8, SG]), op=ALU.subtract),
                                      reads=[t_tab[ts]], writes=[t_thS[sb_]])
                            sy.op("dve", lambda e: e.tensor_tensor(out=w13t[:], in0=w1S[:, sb_, 3, :, :], in1=v2[:, 3, :, 0:1].to_broadcast([128, 8, SG]), op=ALU.add),
                                  reads=[t_tab[3], t_thS[sb_]], writes=[t_w13t])
                            sy.op("act", lambda e: e.activation(out=w13S[:, sb_, :, :].rearrange("p a b -> p (a b)"), in_=w13t[:].rearrange("p a b -> p (a b)"), func=AF.Exp), reads=[t_w13t], writes=[t_thS[sb_]])

                        def st_exp(i1):
                            sg_, il = divmod(i1, SG)
                            sb_ = sg_ % 2
                            if il == 0:
                                sg_tables(sg_)
                            ub = i1 % 4
                            sy.dma(uTb[ub][:].rearrange("p a b -> p (a b)"), UT_bf[i1, :, :], writes=[t_uT[ub]])
                            ei = i1 % 2
                            for ts in range(3):
                                for h in range(8):
                                    sy.op("act", lambda e, ts=ts, h=h: e.activation(out=Ew[ei][:, ts * 8 + h, :], in_=s2[:, ts, h, :], func=AF.Exp, bias=w1S[:, sb_, ts, h, il:il + 1], scale=1.0),
                                          reads=[t_tab[ts], t_thS[sb_]], writes=[t_Ew[ei][ts * 8 + h]])
                            sy.op("dve", lambda e: e.tensor_tensor(out=Ew[ei][:, 24:32, :], in0=E2t[:], in1=w13S[:, sb_, :, il:il + 1].to_broadcast([128, 8, 128]), op=ALU.mult),
                                  reads=[t_E2t, t_thS[sb_]], writes=t_Ew[ei][24:32])

                        def st_m(i1):
                            sg_, il = divmod(i1, SG)
                            sb_ = sg_ % 2
                            ei = i1 % 2
                            for tp in range(2):
                                ai = (i1 % 2) * 2 + tp
                                A = Ab[ai]
                                tsl = slice(2 * tp, 2 * tp + 2)
                                rd = [t_tab[2 * tp], t_tab[2 * tp + 1]]
                                sy.op("dve", lambda e, A=A, tsl=tsl: e.tensor_tensor(out=A[:], in0=s2[:, tsl, :, :].rearrange("p a h i -> p (a h) i"),
                                                                                  in1=thS[:, sb_, tsl, :, il:il + 1].rearrange("p a h o -> p (a h) o").to_broadcast([128, 16, 128]), op=ALU.is_ge),
                                      reads=rd + [t_thS[sb_]], writes=[t_Ab[ai]])
                                sy.op("dve", lambda e, A=A, tp=tp: e.tensor_tensor(out=A[:], in0=A[:], in1=Ew[ei][:, tp * 16:(tp + 1) * 16, :], op=ALU.mult),
                                      reads=[t_Ab[ai]] + t_Ew[ei][tp * 16:(tp + 1) * 16], writes=[t_Ab[ai]])

                        def st_z(i1):
                            ub = i1 % 4
                            pz, t_pz = pf[i1 % 2], t_pf[i1 % 2]
                            for dk in range(16):
                                sy.op("pe", lambda e, dk=dk: e.matmul(pz[:], lhsT=uTb[ub][:, dk, :], rhs=xnT[:, dk, :], start=(dk == 0), stop=(dk == 15)),
                                      reads=[t_uT[ub]] + t_xnT, writes=[t_pz])

                        def st_G(i1):
                            pG, t_pG = pf[2 + i1 % 2], t_pf[2 + i1 % 2]
                            for tp in range(2):
                                ai = (i1 % 2) * 2 + tp
                                A = Ab[ai]
                                for tl in range(2):
                                    ts = 2 * tp + tl
                                    for h in range(8):
                                        sy.op("pe", lambda e, A=A, tl=tl, h=h, ts=ts: e.matmul(pG[:, ts * 128:(ts + 1) * 128], lhsT=A[:, tl * 8 + h, :], rhs=identb[:], start=(h == 0), stop=(h == 7)),
                                              reads=[t_Ab[ai], t_c], writes=[t_pG])

                        def st_gelu(i1):
                            sg_, il = divmod(i1, SG)
                            sb_ = sg_ % 2
                            pG, t_pG = pf[2 + i1 % 2], t_pf[2 + i1 % 2]
                            pz, t_pz = pf[i1 % 2], t_pf[i1 % 2]
                            zb = i1 % 2
                            sy.op("act", lambda e: e.activation(out=gz[zb][:], in_=pz[:], func=AF.Gelu), reads=[t_pz], writes=[t_gz[zb]])
                            sy.op("dve", lambda e: e.tensor_tensor(out=actG[sb_][:, il, :], in0=gz[zb][:], in1=pG[:], op=ALU.mult), reads=[t_gz[zb], t_pG], writes=[t_actG[sb_][il]])
                            if il == SG - 1:
                                for nb in range(4):
                                    f2_queue.append((sg_, nb))

                        f2_queue = []

                        def f2_slice():
                            if not f2_queue:
                                return
                            sg_, nb = f2_queue.pop(0)
                            sb_ = sg_ % 2
                            i1s = slice(sg_ * SG, (sg_ + 1) * SG)
                            vbi = nb % 2
                            sy.dma(vb[vbi][:], PV_v[:, i1s, nb * 512:(nb + 1) * 512], writes=[t_vb[vbi]])
                            for ts in range(4):
                                pO, t_pO = pf[4 + ts], t_pf[4 + ts]
                                for il2 in range(SG):
                                    sy.op("pe", lambda e, il2=il2: e.matmul(pO[:], lhsT=actG[sb_][:, il2, ts * 128:(ts + 1) * 128], rhs=vb[vbi][:, il2, :], start=(il2 == 0), stop=(il2 == SG - 1)),
                                          reads=[t_actG[sb_][il2], t_vb[vbi]], writes=[t_pO])
                                sy.op("dve", lambda e: e.tensor_tensor(out=acc[:, ts, nb * 512:(nb + 1) * 512], in0=pO[:], in1=acc[:, ts, nb * 512:(nb + 1) * 512], op=ALU.add),
                                      reads=[t_pO, t_acc[ts][2 * nb], t_acc[ts][2 * nb + 1]], writes=[t_acc[ts][2 * nb], t_acc[ts][2 * nb + 1]])

                        for i1 in (0, 1):
                            st_exp(i1)
                        for i1 in (0, 1):
                            st_m(i1)
                        for i1 in (0, 1):
                            st_z(i1)
                        for s_ in range(0, 128, 2):
                            st_G(s_)
                            if s_ + 2 < 128:
                                st_exp(s_ + 2)
                                st_m(s_ + 2)
                                st_exp(s_ + 3)
                            st_gelu(s_)
                            st_G(s_ + 1)
                            if s_ + 3 < 128:
                                st_m(s_ + 3)
                            st_gelu(s_ + 1)
                            if s_ + 2 < 128:
                                st_z(s_ + 2)
                                st_z(s_ + 3)
                            f2_slice()
                        while f2_queue:
                            f2_slice()
                        sy.barrier()
                    sy.barrier()

                sy.enabled = "G" in PH
                with ExitStack() as st:
                    gB = sb(st, "gBz", [128, D])
                    ot = [sb(st, "ot%d" % i, [128, D]) for i in range(2)]
                    ssq = [sb(st, "ssqG%d" % i, [128, 1]) for i in range(2)]
                    t_g = Tok(); t_ot = toks(2); t_ssq = toks(2)
                    sy.dma(gB[:], g_fin_d.partition_broadcast(128), writes=[t_g])
                    for ts in range(4):
                        b = ts % 2
                        sy.op("act", lambda e, b=b, ts=ts: e.activation(out=ot[b][:], in_=acc[:, ts, :], func=AF.Square, accum_out=ssq[b][:]), reads=t_acc[ts], writes=[t_ot[b], t_ssq[b]])
                        rms_rstd(ssq[b][:], t_ssq[b], D)
                        sy.op("dve", lambda e, b=b, ts=ts: e.scalar_tensor_tensor(out=ot[b][:], in0=acc[:, ts, :], scalar=ssq[b][:, 0:1], in1=gB[:], op0=ALU.mult, op1=ALU.mult),
                              reads=t_acc[ts] + [t_ssq[b], t_g], writes=[t_ot[b]])
                        r0 = (4 * g + ts) * 128
                        sy.dma(y_d[r0:r0 + 128, :], ot[b][:], reads=[t_ot[b]])
                    sy.barrier()
            sy.barrier()
        sy.finish()
        build_program.ninstr = sy.ninstr
    return nc


_CACHE = {}


def prep_inputs(inp, S):
    f = lambda a: np.ascontiguousarray(np.asarray(a, dtype=np.float32))
    x = f(inp["x"])
    B = x.shape[0]
    q = np.arange(128)
    tri = np.where(q[None, :] <= q[:, None], 0.0, NEG).astype(np.float32)
    bw = 128
    rnnp = np.stack([f(inp["conv_w"])[0, 0], f(inp["conv_w"])[0, 1], f(inp["conv_w"])[0, 2], f(inp["conv_w"])[0, 3],
                     f(inp["conv_b"])[0], f(inp["rg_ba"])[0], f(inp["rg_bx"])[0], f(inp["rg_lambda"])[0]], axis=0)
    rnnp = np.ascontiguousarray(rnnp.reshape(8, 16, 128).transpose(2, 0, 1))
    common = {
        "tri": tri,
        "rnnp": rnnp,
        "g_mix": f(inp["norm_mix_g"])[0], "g_ffn": f(inp["norm_ffn_g"])[0], "g_fin": f(inp["norm_final_g"]),
        "g_kv": f(inp["kv_norm_g"])[0], "idx_g": f(inp["idx_ln_g"])[0], "idx_b": f(inp["idx_ln_b"])[0],
        "w_in": f(inp["w_in"])[0], "w_o": f(inp["w_o"])[0], "wq": f(inp["peer_wq"])[0],
        "wuk": np.ascontiguousarray(f(inp["w_uk"])[0].reshape(16, 4, 128, 128).transpose(0, 2, 1, 3)),
        "wuv": np.ascontiguousarray(f(inp["w_uv"])[0].transpose(1, 0, 2).reshape(512, D)),
        "rgwa": np.ascontiguousarray(f(inp["rg_wa"])[0].transpose(1, 0, 2)),
        "rgwx": np.ascontiguousarray(f(inp["rg_wx"])[0].transpose(1, 0, 2)),
        "k1T": np.ascontiguousarray(f(inp["peer_keys1"])[0].T), "k2T": np.ascontiguousarray(f(inp["peer_keys2"])[0].T),
        "uT": np.ascontiguousarray(f(inp["peer_u"])[0].reshape(128, 128, 16, 128).transpose(0, 3, 2, 1)).reshape(128, 128, 2048),
        "pv": f(inp["peer_v"])[0],
        "pow2": np.ascontiguousarray(np.broadcast_to((0.5 ** np.arange(1, NBIS + 2)).astype(np.float32), (128, NBIS + 1))),
    }
    maps = []
    for b in range(B):
        for hh in range(2):
            m = dict(common)
            if hh == 1:
                m["xs"] = x[b]
                m["kb0"] = np.zeros((128, 128), np.float32)
                m["vrow"] = np.ones((128, 128), np.float32)
            else:
                xs = np.zeros((S, D), np.float32)
                xs[128:] = x[b, :S - 128]
                m["xs"] = xs
                m["kb0"] = np.full((128, 128), NEG, np.float32)
                m["vrow"] = np.zeros((128, 128), np.float32)
            maps.append(m)
    return maps


def assemble(results, B, S):
    out = np.empty((B, S, D), np.float32)
    ov = out.reshape(B, S // 256, 2, 128, D)
    for b in range(B):
        for hh in range(2):
            y = np.asarray(results[b * 2 + hh]["y"]).reshape(S // 256, 128, D)
            ov[b, :, hh] = y
    return out


def kernel(**inputs):
    x = np.asarray(inputs["x"])
    B, S, _ = x.shape
    if S not in _CACHE:
        _CACHE[S] = build_program(S)
    nc = _CACHE[S]
    maps = prep_inputs(inputs, S)
    res = run_bass_kernel_spmd(nc, maps, core_ids=list(range(2 * B)))
    return assemble(res.results, B, S)
```
